# Optimizing a Trainium2 kernel written in Bass

```python
import jax, jax.numpy as jnp
from jax import lax
import numpy as np

D_MODEL = 4096
BATCH = 1
SEQ = 8192
DEPTH = 2

MIX_WIDTH = D_MODEL
POOL_WIDTH = MIX_WIDTH // 4
POOL_WINDOWS = (2, 4, 8, 16)
N_POOL_GROUPS = len(POOL_WINDOWS)
POOL_GROUP = POOL_WIDTH // N_POOL_GROUPS
CONV_WIDTH = MIX_WIDTH // 4
CONV_KERNEL = 31
RWKV_WIDTH = MIX_WIDTH - POOL_WIDTH - CONV_WIDTH
RWKV_HEAD = 64
RWKV_HEADS = RWKV_WIDTH // RWKV_HEAD
DECAY_LORA = max(32, round(1.8 * RWKV_WIDTH ** 0.5 / 32) * 32)
AAA_LORA = max(32, round(1.8 * RWKV_WIDTH ** 0.5 / 32) * 32)
MV_LORA = max(32, round(1.3 * RWKV_WIDTH ** 0.5 / 32) * 32)
GATE_LORA = max(32, round(0.6 * RWKV_WIDTH ** 0.8 / 32) * 32)
RWKV_COLS = 3 * RWKV_WIDTH + DECAY_LORA + AAA_LORA + GATE_LORA
IN_COLS = POOL_WIDTH + 2 * CONV_WIDTH + RWKV_COLS
N_GROUPS = 8
EXPERTS_PER_GROUP = 8
N_EXPERTS = N_GROUPS * EXPERTS_PER_GROUP
TOP_K = 2
EXPERT_FF = 512
MOE_BLOCK = 128
NORM_EPS = 1e-6
LN_EPS = 1e-5
RWKV_GN_EPS = 64e-5

kernel_name = 'hybrid_pool_conv_rwkv7_hmoe'


def rms_norm(x, gain):
    xf = x.astype(jnp.float32)
    y = xf * lax.rsqrt(jnp.mean(xf * xf, axis=-1, keepdims=True) + NORM_EPS)
    return (y * gain.astype(jnp.float32)).astype(x.dtype)


def layer_norm(x, gain, bias):
    xf = x.astype(jnp.float32)
    mu = jnp.mean(xf, axis=-1, keepdims=True)
    var = jnp.mean(jnp.square(xf - mu), axis=-1, keepdims=True)
    y = (xf - mu) * lax.rsqrt(var + LN_EPS)
    return (y * gain.astype(jnp.float32) + bias.astype(jnp.float32)).astype(x.dtype)


def token_shift(p, mix):
    prev = jnp.pad(p, ((0, 0), (1, 0), (0, 0)))[:, :-1]
    return p + (prev - p) * mix


def pool_mixer(u, w, scale):
    b, t, _ = u.shape
    uf = u.astype(jnp.float32).reshape(b, t, N_POOL_GROUPS, POOL_GROUP)
    cs = jnp.pad(jnp.cumsum(uf, axis=1), ((0, 0), (1, 0), (0, 0), (0, 0)))
    pos = jnp.arange(t)[:, None]
    win = jnp.asarray(POOL_WINDOWS, dtype=jnp.int32)[None, :]
    lo = jnp.maximum(pos + 1 - win, 0)
    grp = jnp.arange(N_POOL_GROUPS)[None, :]
    window_sum = cs[:, 1:] - cs[:, lo, grp]
    count = jnp.minimum(pos + 1, win).astype(jnp.float32)
    mixed = window_sum / count[None, :, :, None] - uf
    y = jnp.einsum('btgc,gcd->btgd', mixed, w.astype(jnp.float32))
    return (y.reshape(b, t, POOL_WIDTH) * scale.astype(jnp.float32)).astype(u.dtype)


def conv_module(u, dw, dw_b, ln_g, ln_b, pw):
    val, gate = jnp.split(u, 2, axis=-1)
    h = val * jax.nn.sigmoid(gate)
    h = lax.conv_general_dilated(h, dw[:, None, :], window_strides=(1,),
                                 padding=[(CONV_KERNEL - 1, 0)],
                                 dimension_numbers=('NWC', 'WIO', 'NWC'),
                                 feature_group_count=CONV_WIDTH) + dw_b
    h = jax.nn.silu(layer_norm(h, ln_g, ln_b))
    return h @ pw


def rwkv7_recurrence(r, decay, k, v, a, b):
    bsz, t, h, n = r.shape

    def step(state, inp):
        r_t, w_t, k_t, v_t, a_t, b_t = inp
        sa = jnp.einsum('bhvk,bhk->bhv', state, a_t)
        state = (state * w_t[:, :, None, :] + sa[..., None] * b_t[:, :, None, :]
                 + v_t[..., None] * k_t[:, :, None, :])
        return state, jnp.einsum('bhvk,bhk->bhv', state, r_t)

    seqs = tuple(jnp.moveaxis(z.astype(jnp.float32), 1, 0) for z in (r, decay, k, v, a, b))
    state0 = jnp.zeros((bsz, h, n, n), jnp.float32)
    _, y = lax.scan(step, state0, seqs)
    return jnp.moveaxis(y, 0, 1)


def rwkv7_mixer(p, w0, w_up, a0, a_up, g_up, k_k, k_a, r_k, ln_g, ln_b, v_first, v_res):
    bsz, t, _ = p.shape
    c = RWKV_WIDTH
    r, k, v = p[..., :c], p[..., c:2 * c], p[..., 2 * c:3 * c]
    o = 3 * c
    xw = p[..., o:o + DECAY_LORA]
    o += DECAY_LORA
    xa = p[..., o:o + AAA_LORA]
    o += AAA_LORA
    xg = p[..., o:o + GATE_LORA]
    w_log = -jax.nn.softplus(-(w0 + jnp.tanh(xw) @ w_up).astype(jnp.float32)) - 0.5
    decay = jnp.exp(-jnp.exp(w_log))
    a = jax.nn.sigmoid(a0 + xa @ a_up)
    g = jax.nn.sigmoid(xg) @ g_up
    if v_res is None:
        v_first = v
    else:
        vd, v0, v_up = v_res
        v = v + (v_first - v) * jax.nn.sigmoid(v0 + vd @ v_up)

    def heads(z):
        return z.reshape(bsz, t, RWKV_HEADS, RWKV_HEAD).astype(jnp.float32)

    kk = heads(k * k_k)
    kk = kk * lax.rsqrt(jnp.maximum(jnp.sum(kk * kk, axis=-1, keepdims=True), 1e-24))
    k = k * (1 + (a - 1) * k_a)
    rh, kh, vh = heads(r), heads(k), heads(v)
    y = rwkv7_recurrence(rh, heads(decay), kh, vh, -kk, kk * heads(a))
    mu = jnp.mean(y, axis=-1, keepdims=True)
    var = jnp.mean(jnp.square(y - mu), axis=-1, keepdims=True)
    y = ((y - mu) * lax.rsqrt(var + RWKV_GN_EPS)).reshape(bsz, t, c)
    y = y * ln_g.astype(jnp.float32) + ln_b.astype(jnp.float32)
    bonus = jnp.sum(rh * kh * r_k.astype(jnp.float32), axis=-1, keepdims=True) * vh
    y = y + bonus.reshape(bsz, t, c)
    return (y * g.astype(jnp.float32)).astype(p.dtype), v_first


def hier_moe(h, wg, bg, we, be, w_in, w_out):
    bsz, t, d = h.shape
    nt = bsz * t
    ht = h.reshape(nt, d)
    g_prob = jax.nn.softmax((ht @ wg).astype(jnp.float32) + bg.astype(jnp.float32), axis=-1)
    p_grp, grp = lax.top_k(g_prob, 1)
    e_all = ((ht @ we).astype(jnp.float32) + be.astype(jnp.float32)).reshape(nt, N_GROUPS, EXPERTS_PER_GROUP)
    e_prob = jax.nn.softmax(e_all[jnp.arange(nt), grp[:, 0]], axis=-1)
    top_p, top_i = lax.top_k(e_prob, TOP_K)
    gate = p_grp * top_p / jnp.sum(top_p, axis=-1, keepdims=True)
    eid = (grp * EXPERTS_PER_GROUP + top_i).reshape(-1)
    tok = jnp.repeat(jnp.arange(nt, dtype=jnp.int32), TOP_K)
    gate = gate.reshape(-1)
    n_slots = nt * TOP_K
    order = jnp.argsort(eid)
    e_sorted, tok_sorted, gate_sorted = eid[order], tok[order], gate[order]
    counts = jnp.bincount(eid, length=N_EXPERTS)
    padded = (counts + MOE_BLOCK - 1) // MOE_BLOCK * MOE_BLOCK
    pad_end = jnp.cumsum(padded)
    pad_start = pad_end - padded
    start = jnp.cumsum(counts) - counts
    dest = pad_start[e_sorted] + jnp.arange(n_slots) - start[e_sorted]
    n_blocks = -(-(n_slots + N_EXPERTS * (MOE_BLOCK - 1)) // MOE_BLOCK)
    slot_tok = jnp.full((n_blocks * MOE_BLOCK,), nt, jnp.int32).at[dest].set(tok_sorted)
    slot_gate = jnp.zeros((n_blocks * MOE_BLOCK,), jnp.float32).at[dest].set(gate_sorted)
    block_e = jnp.minimum(jnp.searchsorted(pad_end, jnp.arange(n_blocks) * MOE_BLOCK, side='right'),
                          N_EXPERTS - 1)
    h_pad = jnp.concatenate([ht, jnp.zeros((1, d), ht.dtype)], axis=0)

    def expert_block(args):
        rows, e = args
        xb = h_pad[rows]
        gt, up = jnp.split(xb @ w_in[e], 2, axis=-1)
        return (jax.nn.silu(gt) * up) @ w_out[e]

    yb = lax.map(expert_block, (slot_tok.reshape(n_blocks, MOE_BLOCK), block_e))
    yb = yb.reshape(-1, d) * slot_gate[:, None].astype(yb.dtype)
    y = jax.ops.segment_sum(yb, slot_tok, num_segments=nt + 1)[:nt]
    return y.reshape(bsz, t, d)


def setup_inputs(seed: int = 0) -> dict:
    key = jax.random.key(seed)
    ks = iter(jax.random.split(key, 40))

    def nrm(shape, scale):
        return jax.random.normal(next(ks), shape, jnp.float32) * scale

    def unif(shape, lo, hi):
        return jax.random.uniform(next(ks), shape, jnp.float32, lo, hi)

    L, Lv = DEPTH, DEPTH - 1
    return {
        'x': nrm((BATCH, SEQ, D_MODEL), 1.0),
        'mix_norm': 1.0 + nrm((L, D_MODEL), 0.02),
        'w_in': nrm((L, D_MODEL, IN_COLS), D_MODEL ** -0.5),
        'shift_mix': unif((L, RWKV_COLS), 0.0, 1.0),
        'pool_w': nrm((L, N_POOL_GROUPS, POOL_GROUP, POOL_GROUP), POOL_GROUP ** -0.5),
        'pool_scale': 1.0 + nrm((L, POOL_WIDTH), 0.1),
        'conv_dw': nrm((L, CONV_KERNEL, CONV_WIDTH), CONV_KERNEL ** -0.5),
        'conv_dw_b': nrm((L, CONV_WIDTH), 0.02),
        'conv_ln_g': 1.0 + nrm((L, CONV_WIDTH), 0.02),
        'conv_ln_b': nrm((L, CONV_WIDTH), 0.02),
        'conv_pw': nrm((L, CONV_WIDTH, CONV_WIDTH), CONV_WIDTH ** -0.5),
        'rwkv_w0': unif((L, RWKV_WIDTH), -4.0, 1.0),
        'rwkv_w_up': nrm((L, DECAY_LORA, RWKV_WIDTH), 0.5 * DECAY_LORA ** -0.5),
        'rwkv_a0': nrm((L, RWKV_WIDTH), 0.1),
        'rwkv_a_up': nrm((L, AAA_LORA, RWKV_WIDTH), 0.5 * AAA_LORA ** -0.5),
        'rwkv_g_up': nrm((L, GATE_LORA, RWKV_WIDTH), GATE_LORA ** -0.5),
        'rwkv_k_k': 0.85 + nrm((L, RWKV_WIDTH), 0.05),
        'rwkv_k_a': 1.0 + nrm((L, RWKV_WIDTH), 0.05),
        'rwkv_r_k': nrm((L, RWKV_HEADS, RWKV_HEAD), 0.1),
        'rwkv_ln_g': 1.0 + nrm((L, RWKV_WIDTH), 0.02),
        'rwkv_ln_b': nrm((L, RWKV_WIDTH), 0.02),
        'rwkv_v_down': nrm((Lv, D_MODEL, MV_LORA), D_MODEL ** -0.5),
        'rwkv_v_shift': unif((Lv, MV_LORA), 0.0, 1.0),
        'rwkv_v0': nrm((Lv, RWKV_WIDTH), 0.5),
        'rwkv_v_up': nrm((Lv, MV_LORA, RWKV_WIDTH), 0.5 * MV_LORA ** -0.5),
        'w_out': nrm((L, MIX_WIDTH, D_MODEL), MIX_WIDTH ** -0.5),
        'ffn_norm': 1.0 + nrm((L, D_MODEL), 0.02),
        'router_group_w': nrm((L, D_MODEL, N_GROUPS), D_MODEL ** -0.5),
        'router_group_b': nrm((L, N_GROUPS), 0.01),
        'router_expert_w': nrm((L, D_MODEL, N_EXPERTS), D_MODEL ** -0.5),
        'router_expert_b': nrm((L, N_EXPERTS), 0.01),
        'expert_w_in': nrm((L, N_EXPERTS, D_MODEL, 2 * EXPERT_FF), D_MODEL ** -0.5),
        'expert_w_out': nrm((L, N_EXPERTS, EXPERT_FF, D_MODEL), EXPERT_FF ** -0.5),
        'final_norm': 1.0 + nrm((D_MODEL,), 0.02),
    }


def reference(x, mix_norm, w_in, shift_mix, pool_w, pool_scale, conv_dw, conv_dw_b, conv_ln_g,
              conv_ln_b, conv_pw, rwkv_w0, rwkv_w_up, rwkv_a0, rwkv_a_up, rwkv_g_up, rwkv_k_k,
              rwkv_k_a, rwkv_r_k, rwkv_ln_g, rwkv_ln_b, rwkv_v_down, rwkv_v_shift, rwkv_v0,
              rwkv_v_up, w_out, ffn_norm, router_group_w, router_group_b, router_expert_w,
              router_expert_b, expert_w_in, expert_w_out, final_norm):
    v_first = None
    for i in range(DEPTH):
        xn = rms_norm(x, mix_norm[i])
        proj = xn @ w_in[i]
        u_pool = proj[..., :POOL_WIDTH]
        u_conv = proj[..., POOL_WIDTH:POOL_WIDTH + 2 * CONV_WIDTH]
        u_rwkv = token_shift(proj[..., POOL_WIDTH + 2 * CONV_WIDTH:], shift_mix[i])
        if i == 0:
            v_res = None
        else:
            vd = token_shift(xn @ rwkv_v_down[i - 1], rwkv_v_shift[i - 1])
            v_res = (vd, rwkv_v0[i - 1], rwkv_v_up[i - 1])
        y_pool = pool_mixer(u_pool, pool_w[i], pool_scale[i])
        y_conv = conv_module(u_conv, conv_dw[i], conv_dw_b[i], conv_ln_g[i], conv_ln_b[i], conv_pw[i])
        y_rwkv, v_first = rwkv7_mixer(u_rwkv, rwkv_w0[i], rwkv_w_up[i], rwkv_a0[i], rwkv_a_up[i],
                                      rwkv_g_up[i], rwkv_k_k[i], rwkv_k_a[i], rwkv_r_k[i],
                                      rwkv_ln_g[i], rwkv_ln_b[i], v_first, v_res)
        x = x + jnp.concatenate([y_pool, y_conv, y_rwkv], axis=-1) @ w_out[i]
        x = x + hier_moe(rms_norm(x, ffn_norm[i]), router_group_w[i], router_group_b[i],
                         router_expert_w[i], router_expert_b[i], expert_w_in[i], expert_w_out[i])
    return rms_norm(x, final_norm)
```

```python
import numpy as np
from contextlib import ExitStack
import concourse.bass as bass
import concourse.mybir as mybir
from concourse.bass_utils import run_bass_kernel_spmd

F32 = mybir.dt.float32
BF16 = mybir.dt.bfloat16
I32 = mybir.dt.int32
AF = mybir.ActivationFunctionType
ALU = mybir.AluOpType


class Prog:
    EPOCH = 12000
    NDS = 40

    def __init__(self, nc, es):
        self.nc, self.es = nc, es
        self.es_root = es
        self.E = {'pe': nc.tensor, 'act': nc.scalar, 'dve': nc.vector,
                  'pool': nc.gpsimd, 'sp': nc.sync}
        self.cur = {}
        self.seen = {e: {} for e in self.E}
        self.bufs = {}
        self.nsem = 0
        self.dsems = []
        self.dcnt = []
        self.di = 0
        self.out_toks = []
        self.ninstr = 0

    def _newsem(self):
        s = self.es_root.enter_context(self.nc.semaphore(f"s{self.nsem}"))
        self.nsem += 1
        return s

    def sb(self, name, shape, dt):
        self.nten = getattr(self, 'nten', 0) + 1
        return self.es.enter_context(self.nc.sbuf_tensor(f"{name}_u{self.nten}", shape, dt))

    def ps(self, name, shape, dt=F32):
        self.nten = getattr(self, 'nten', 0) + 1
        return self.es.enter_context(self.nc.psum_tensor(f"{name}_u{self.nten}", shape, dt))

    def _tok(self, e):
        if e not in self.cur or self.cur[e][1] >= self.EPOCH:
            self.cur[e] = [self._newsem(), 0]
        self.cur[e][1] += 1
        return (self.cur[e][0], self.cur[e][1])

    def _wait(self, e, tok):
        sem, val = tok
        k = id(sem)
        if self.seen[e].get(k, 0) >= val:
            return
        self.E[e].wait_ge(sem, val)
        self.ninstr += 1
        self.seen[e][k] = val

    def _deps(self, e, reads, writes):
        toks = []
        for k in reads:
            b = self.bufs.get(k)
            if b and b['w']:
                toks.append(b['w'])
        for k in writes:
            b = self.bufs.get(k)
            if b:
                if b['w']:
                    toks.append(b['w'])
                toks += list(b['r'].values())
        own = self.cur.get(e)
        for t in toks:
            if e == 'pe' and own is not None and t[0] is own[0]:
                continue
            self._wait(e, t)

    def _commit(self, tok, reads, writes):
        for k in reads:
            b = self.bufs.setdefault(k, {'w': None, 'r': {}})
            o = b['r'].get(id(tok[0]))
            if o is None or o[1] < tok[1]:
                b['r'][id(tok[0])] = tok
        for k in writes:
            self.bufs[k] = {'w': tok, 'r': {}}

    def op(self, e, fn, reads=(), writes=()):
        self._deps(e, reads, writes)
        tok = self._tok(e)
        fn(self.E[e]).then_inc(tok[0], 1)
        self.ninstr += 1
        self._commit(tok, reads, writes)
        return tok

    def dma(self, q, out, in_, reads=(), writes=(), is_output=False, **kw):
        self._deps(q, reads, writes)
        if len(self.dsems) < self.NDS:
            self.dsems.append(self._newsem())
            self.dcnt.append(0)
            i = len(self.dsems) - 1
        else:
            i = self.di
            self.di = (self.di + 1) % self.NDS
            self._wait(q, (self.dsems[i], self.dcnt[i]))
        self.dcnt[i] += 16
        tok = (self.dsems[i], self.dcnt[i])
        self.E[q].dma_start(out=out, in_=in_, **kw).then_inc(tok[0], 16)
        self.ninstr += 1
        self._commit(tok, reads, writes)
        if is_output:
            self.out_toks.append(tok)
        return tok

    def finish(self, q='sp'):
        for t in self.out_toks:
            self._wait(q, t)

    def barrier(self):
        toks = [(v[0], v[1]) for v in self.cur.values()] + [(s_, c) for s_, c in zip(self.dsems, self.dcnt) if c > 0]
        for e in self.E:
            for t in toks:
                self._wait(e, t)
        self.bufs = {}

    def scope(self):
        return _Scope(self)


class _Scope:
    def __init__(self, P):
        self.P = P

    def __enter__(self):
        from contextlib import ExitStack
        self.old = self.P.es
        self.P.es = ExitStack()
        return self

    def __exit__(self, *a):
        self.P.barrier()
        self.P.es.close()
        self.P.es = self.old
        return False


def _idma(self, out, in_, out_idx=None, in_idx=None, reads=(), writes=(), is_output=False):
    q = 'pool'
    self._deps(q, reads, writes)
    if len(self.dsems) < self.NDS:
        self.dsems.append(self._newsem()); self.dcnt.append(0)
        i = len(self.dsems) - 1
    else:
        i = self.di
        self.di = (self.di + 1) % self.NDS
        self._wait(q, (self.dsems[i], self.dcnt[i]))
    self.dcnt[i] += 16
    tok = (self.dsems[i], self.dcnt[i])
    self.nc.gpsimd.indirect_dma_start(
        out=out, out_offset=(bass.IndirectOffsetOnAxis(ap=out_idx, axis=0) if out_idx is not None else None),
        in_=in_, in_offset=(bass.IndirectOffsetOnAxis(ap=in_idx, axis=0) if in_idx is not None else None)).then_inc(tok[0], 16)
    self.ninstr += 1
    self._commit(tok, reads, writes)
    if is_output:
        self.out_toks.append(tok)
    return tok


Prog.idma = _idma


D = 4096
KC = 32
HALO = 32
TB = 1024
NCT = 15
KW = 31
HPC = 4
CH = 32
G = CH // 8
EPS = 1e-6
GN_EPS = 64e-5
T_POOL, T_CV, T_CG, T_XW, T_XA, T_XG0, T_XG1, T_VD, T_R0 = 0, 2, 3, 4, 5, 6, 7, 8, 9


def build_l1(T, layer1):
    nc = bass.Bass("TRN2", target_bir_lowering=False)
    NBLK = T // TB
    TE = TB + HALO
    din = lambda n, s, dt=F32: nc.dram_tensor(n, s, dt, kind="ExternalInput").ap()
    dout = lambda n, s, dt=F32: nc.dram_tensor(n, s, dt, kind="ExternalOutput").ap()
    dscr = lambda n, s, dt=F32: nc.dram_tensor(n, s, dt).ap()
    xT = din("xT", [D, T]); gain = din("gain", [128, KC]); wt = din("wt", [NCT, 128, KC, 128])
    consts = din("consts", [4, 128, 128])
    pwt = din("pwt", [128, 2, 128]); pcol = din("pcol", [128, 5]); pinv = din("pinv", [128, 4, 16])
    dwT = din("dwT", [128, KW + 1])
    mixc = din("mixc", [128, 11]); cols = din("cols", [128, 8, 2]); ups = din("ups", [2, 128, 5, 128])
    lncols = din("lncols", [128, 2, 2])
    vfT = din("vfT", [256, T]) if layer1 else None
    ypoolT = dout("ypoolT", [128, T]); hcT = dout("hcT", [128, T]); yrwT = dout("yrwT", [256, T]); vT = dout("vT", [256, T])
    XG = dscr("XG", [128, KC, T], BF16)
    PS = dscr("PS", [NCT * 128, HALO + T])
    ATM = dscr("ATM", [T, HPC, 64], BF16); BTM = dscr("BTM", [T, HPC, 64], BF16)
    KRV = dscr("KRV", [HPC, T, 129], BF16)
    RPT = dscr("RPT", [64, HPC, T]); WTS = dscr("WTS", [64, HPC, T])
    BONT = dscr("BONT", [256, T]); GT = dscr("GT", [256, T])
    YTM = dscr("YTM", [HPC, T, 64], BF16)
    es = ExitStack(); P = Prog(nc, es)
    RSTD = P.sb("RSTD", [128, T], F32)
    CONST = P.sb("CONST", [128, 4, 128], F32)
    IDB = P.sb("IDB", [128, 128], BF16)
    P.dma('sp', CONST[:], consts.rearrange("c p n -> p c n"), writes=['CONST'])
    P.op('dve', lambda e: e.tensor_copy(out=IDB[:], in_=CONST[:, 3, :]), reads=['CONST'], writes=['IDB'])
    ONES, BONES, BONES64, IDENT = CONST[:, 0, :], CONST[:, 1, :], CONST[:, 2, :], CONST[:, 3, :]

    with P.scope():
        XGs = P.sb("XGs", [128, KC, TB], BF16)
        XC = [P.sb(f"XC{i}", [128, TB], F32) for i in range(2)]
        SQ = [P.sb(f"SQ{i}", [128, TB], F32) for i in range(2)]
        GN = P.sb("GN", [128, KC], F32); EPSC = P.sb("EPSC", [128, 1], F32)
        psA = [P.ps(f"psA{b}", [128, 512]) for b in range(2)]
        P.dma('sp', GN[:], gain[:, :], writes=['GN'])
        P.op('dve', lambda e: e.memset(EPSC[:], EPS), writes=['EPSC'])
        for tb in range(NBLK):
            tsl = slice(tb * TB, (tb + 1) * TB)
            for kc in range(KC):
                i = kc % 2
                P.dma('sp' if i == 0 else 'act', XC[i][:], xT[kc * 128:(kc + 1) * 128, tsl], writes=[f"XC{i}"])
                P.op('act', lambda e: e.activation(out=SQ[i][:], in_=XC[i][:], func=AF.Square), reads=[f"XC{i}"], writes=[f"SQ{i}"])
                for b in range(2):
                    P.op('pe', lambda e: e.matmul(psA[b][:, :], lhsT=ONES, rhs=SQ[i][:, b * 512:(b + 1) * 512],
                                                  start=(kc == 0), stop=(kc == KC - 1)), reads=['CONST', f"SQ{i}"], writes=[f"psA{b}"])
                P.op('dve' if i == 0 else 'pool',
                     lambda e: e.tensor_scalar(out=XGs[:, kc, :], in0=XC[i][:], scalar1=GN[:, kc:kc + 1], scalar2=None, op0=ALU.mult),
                     reads=[f"XC{i}", 'GN'], writes=[f"XGs{kc}"])
            for b in range(2):
                sl = slice(tb * TB + b * 512, tb * TB + (b + 1) * 512)
                P.op('act', lambda e: e.activation(out=RSTD[:, sl], in_=psA[b][:, :], func=AF.Ln, bias=EPSC[:, 0:1], scale=1.0 / D),
                     reads=[f"psA{b}", 'EPSC'], writes=['RSTD'])
            P.op('act', lambda e: e.activation(out=RSTD[:, tsl], in_=RSTD[:, tsl], func=AF.Exp, scale=-0.5), reads=['RSTD'], writes=['RSTD'])
            P.dma('pool', XG[:, :, tsl], XGs[:], reads=[f"XGs{k_}" for k_ in range(KC)], writes=["XGd"])
    with P.scope():
        WF = [P.sb(f"WF{i}", [128, KC, 128], F32) for i in range(2)]
        WB = [P.sb(f"WB{i}", [128, KC, 128], BF16) for i in range(4)]
        XB = [P.sb(f"XB{i}", [128, KC, 512], BF16) for i in range(2)]
        OT = [P.sb(f"OT{i}", [128, 512], F32) for i in range(4)]
        ZT = P.sb("ZT", [128, HALO], F32)
        psB = [P.ps(f"psB{i}", [128, 512]) for i in range(4)]
        P.op('dve', lambda e: e.memset(ZT[:], 0.0), writes=['ZT'])
        for ct in range(NCT):
            P.dma('sp', PS[ct * 128:(ct + 1) * 128, 0:HALO], ZT[:], reads=['ZT'], writes=[f"PSd{ct}"])
        n = 0
        for g0 in range(0, NCT, 4):
            cts = list(range(g0, min(g0 + 4, NCT)))
            for qi, ct in enumerate(cts):
                i = qi % 2
                P.dma('act', WF[i][:], wt[ct], writes=[f"WF{i}"])
                P.op('pool', lambda e: e.tensor_copy(out=WB[qi][:], in_=WF[i][:]), reads=[f"WF{i}"], writes=[f"WB{qi}"])
            for blk in range(T // 512):
                xi = blk % 2
                bsl = slice(blk * 512, (blk + 1) * 512)
                P.dma('sp', XB[xi][:], XG[:, :, bsl], reads=["XGd"], writes=[f"XB{xi}"])
                for qi, ct in enumerate(cts):
                    pb = n % 4; n += 1
                    for kc in range(KC):
                        P.op('pe', lambda e: e.matmul(psB[pb][:, :], lhsT=WB[qi][:, kc, :], rhs=XB[xi][:, kc, :],
                                                      start=(kc == 0), stop=(kc == KC - 1)),
                             reads=[f"WB{qi}", f"XB{xi}"] if kc == 0 else [], writes=[f"psB{pb}"])
                    P.op('dve', lambda e: e.tensor_tensor(out=OT[pb][:], in0=psB[pb][:, :], in1=RSTD[:, bsl], op=ALU.mult),
                         reads=[f"psB{pb}", 'RSTD'], writes=[f"OT{pb}"])
                    P.dma('pool', PS[ct * 128:(ct + 1) * 128, HALO + blk * 512:HALO + (blk + 1) * 512], OT[pb][:],
                          reads=[f"OT{pb}"], writes=[f"PSd{ct}"])
                P._commit((P.cur['pe'][0], P.cur['pe'][1]), [f"XB{xi}"] + [f"WB{q}" for q in range(len(cts))], [])
    psd = [f"PSd{ct}" for ct in range(NCT)]
    with P.scope():
        U = [P.sb(f"U{i}", [128, TE], F32) for i in range(2)]
        SW = [P.sb(f"SW{i}", [128, TE], F32) for i in range(4)]
        TMP = P.sb("TMP", [128, TB], F32); T16 = P.sb("T16", [128, 16], F32)
        MX = [P.sb(f"MX{i}", [128, TB], BF16) for i in range(2)]
        PWF = P.sb("PWF", [128, 2, 128], F32); PWB = P.sb("PWB", [128, 2, 128], BF16)
        PCOL = P.sb("PCOL", [128, 5], F32); PINV = P.sb("PINV", [128, 4, 16], F32)
        OTp = P.sb("OTp", [128, TB], F32)
        psP = [P.ps(f"psP{i}", [128, 512]) for i in range(2)]
        P.dma('sp', PWF[:], pwt[:, :, :], writes=['PWF']); P.dma('sp', PCOL[:], pcol[:, :], writes=['PCOL'])
        P.dma('sp', PINV[:], pinv[:, :, :], writes=['PINV'])
        P.op('pool', lambda e: e.tensor_copy(out=PWB[:], in_=PWF[:]), reads=['PWF'], writes=['PWB'])
        for tb in range(NBLK):
            for c2 in range(2):
                P.dma('sp', U[c2][:], PS[(T_POOL + c2) * 128:(T_POOL + c2 + 1) * 128, tb * TB:tb * TB + TE], reads=psd, writes=[f"U{c2}"])
                src, skey = U[c2], f"U{c2}"
                for st in range(4):
                    sh = 1 << st
                    P.op('dve' if st % 2 == 0 else 'pool',
                         lambda e: e.tensor_tensor(out=SW[st][:, sh:TE], in0=src[:, sh:TE], in1=src[:, 0:TE - sh], op=ALU.add),
                         reads=[skey], writes=[f"SW{st}"])
                    src, skey = SW[st], f"SW{st}"
                P.op('dve', lambda e: e.tensor_scalar(out=TMP[:], in0=SW[0][:, HALO:TE], scalar1=PCOL[:, 0:1], scalar2=None, op0=ALU.mult),
                     reads=["SW0", 'PCOL'], writes=['TMP'])
                for st in range(1, 4):
                    P.op('dve', lambda e: e.scalar_tensor_tensor(out=TMP[:], in0=SW[st][:, HALO:TE], scalar=PCOL[:, st:st + 1], in1=TMP[:],
                                                                 op0=ALU.mult, op1=ALU.add), reads=[f"SW{st}", 'PCOL', 'TMP'], writes=['TMP'])
                if tb == 0:
                    P.op('dve', lambda e: e.tensor_tensor(out=TMP[:, 0:16], in0=SW[0][:, HALO:HALO + 16], in1=PINV[:, 0, :], op=ALU.mult),
                         reads=["SW0", 'PINV', 'TMP'], writes=['TMP'])
                    for st in range(1, 4):
                        P.op('dve', lambda e: e.tensor_tensor(out=T16[:], in0=SW[st][:, HALO:HALO + 16], in1=PINV[:, st, :], op=ALU.mult),
                             reads=[f"SW{st}", 'PINV'], writes=['T16'])
                        P.op('dve', lambda e: e.tensor_tensor(out=TMP[:, 0:16], in0=TMP[:, 0:16], in1=T16[:], op=ALU.add),
                             reads=['TMP', 'T16'], writes=['TMP'])
                P.op('dve', lambda e: e.tensor_tensor(out=MX[c2][:], in0=TMP[:], in1=U[c2][:, HALO:TE], op=ALU.subtract),
                     reads=['TMP', f"U{c2}"], writes=[f"MX{c2}"])
            for b in range(2):
                for c2 in range(2):
                    P.op('pe', lambda e: e.matmul(psP[b][:, :], lhsT=PWB[:, c2, :], rhs=MX[c2][:, b * 512:(b + 1) * 512],
                                                  start=(c2 == 0), stop=(c2 == 1)), reads=['PWB', f"MX{c2}"], writes=[f"psP{b}"])
                P.op('act', lambda e: e.activation(out=OTp[:, b * 512:(b + 1) * 512], in_=psP[b][:, :], func=AF.Copy, scale=PCOL[:, 4:5]),
                     reads=[f"psP{b}", 'PCOL'], writes=['OTp'])
            P.dma('pool', ypoolT[:, tb * TB:(tb + 1) * TB], OTp[:], reads=['OTp'], is_output=True)
    with P.scope():
        V = [P.sb(f"V{i}", [128, TE], F32) for i in range(2)]
        Gt = [P.sb(f"G{i}", [128, TE], F32) for i in range(2)]
        H = [P.sb(f"H{i}", [128, TE], F32) for i in range(2)]
        HC = [P.sb(f"HC{i}", [128, TB], F32) for i in range(2)]
        DW = P.sb("DW", [128, KW + 1], F32)
        P.dma('sp', DW[:], dwT[:, :], writes=['DW'])
        o0 = HALO - (KW - 1)
        for tb in range(NBLK):
            i = tb % 2
            P.dma('sp', V[i][:], PS[T_CV * 128:(T_CV + 1) * 128, tb * TB:tb * TB + TE], reads=psd, writes=[f"V{i}"])
            P.dma('act', Gt[i][:], PS[T_CG * 128:(T_CG + 1) * 128, tb * TB:tb * TB + TE], reads=psd, writes=[f"G{i}"])
            P.op('act', lambda e: e.activation(out=Gt[i][:], in_=Gt[i][:], func=AF.Sigmoid), reads=[f"G{i}"], writes=[f"G{i}"])
            P.op('pool', lambda e: e.tensor_tensor(out=H[i][:], in0=V[i][:], in1=Gt[i][:], op=ALU.mult), reads=[f"V{i}", f"G{i}"], writes=[f"H{i}"])
            P.op('dve', lambda e: e.tensor_scalar(out=HC[i][:], in0=H[i][:, o0:o0 + TB], scalar1=DW[:, 0:1], scalar2=DW[:, KW:KW + 1],
                                                  op0=ALU.mult, op1=ALU.add), reads=[f"H{i}", 'DW'], writes=[f"HC{i}"])
            for jj in range(1, KW):
                P.op('dve', lambda e: e.scalar_tensor_tensor(out=HC[i][:], in0=H[i][:, o0 + jj:o0 + jj + TB], scalar=DW[:, jj:jj + 1],
                                                             in1=HC[i][:], op0=ALU.mult, op1=ALU.add),
                     reads=[f"H{i}", 'DW', f"HC{i}"], writes=[f"HC{i}"])
            P.dma('pool', hcT[:, tb * TB:(tb + 1) * TB], HC[i][:], reads=[f"HC{i}"], is_output=True)
    with P.scope():
        PT = [P.sb(f"PT{i}", [128, TE], F32) for i in range(3)]
        NL = 5
        LO = [P.sb(f"LO{i}", [128, TB], BF16) for i in range(NL)]
        names = ["R", "K", "V", "D", "E1", "Wd", "A", "G", "KK", "T1", "T2", "AA", "BBv", "Kp", "Rp", "RK", "BON"]
        Ft = {nn: P.sb("F_" + nn, [128, TB], F32) for nn in names}
        UPF = P.sb("UPF", [128, 5, 128], F32); UPB = [P.sb(f"UPB{j}", [128, 5, 128], BF16) for j in range(2)]
        MIX = P.sb("MIX", [128, 11], F32); COL = P.sb("COL", [128, 8, 2], F32)
        TMA = P.sb("TMA", [128, 8, 2, 64], BF16); TMB = P.sb("TMB", [128, 8, 2, 64], BF16); TMK = P.sb("TMK", [128, 8, 2, 129], BF16)
        ps = [P.ps(f"ps{i}", [128, 512]) for i in range(8)]
        P.dma('sp', MIX[:], mixc[:, :], writes=['MIX']); P.dma('sp', COL[:], cols[:, :, :], writes=['COL'])
        P.op('dve', lambda e: e.tensor_scalar(out=COL[:, 0, :], in0=COL[:, 0, :], scalar1=-1.0, scalar2=None, op0=ALU.mult), reads=['COL'], writes=['COL'])
        P.op('dve', lambda e: e.tensor_scalar(out=COL[:, 4, :], in0=COL[:, 3, :], scalar1=-1.0, scalar2=1.0, op0=ALU.mult, op1=ALU.add), reads=['COL'], writes=['COL'])
        for j in range(2):
            P.dma('act', UPF[:], ups[j], writes=['UPF'])
            P.op('pool', lambda e: e.tensor_copy(out=UPB[j][:], in_=UPF[:]), reads=['UPF'], writes=[f"UPB{j}"])
        pc = [0]

        def nps():
            pc[0] = (pc[0] + 1) % 8
            return pc[0]

        def shift(dst, dkey, i, mcol):
            P.op('pool', lambda e: e.tensor_tensor(out=Ft["D"][:], in0=PT[i][:, HALO - 1:TE - 1], in1=PT[i][:, HALO:TE], op=ALU.subtract),
                 reads=[f"PT{i}"], writes=["D"])
            P.op('dve', lambda e: e.scalar_tensor_tensor(out=dst[:], in0=Ft["D"][:], scalar=MIX[:, mcol:mcol + 1], in1=PT[i][:, HALO:TE],
                                                         op0=ALU.mult, op1=ALU.add), reads=["D", f"PT{i}", 'MIX'], writes=[dkey])

        def bsum(src, skey):
            res = []
            for b in range(2):
                pb = nps()
                P.op('pe', lambda e: e.matmul(ps[pb][:, :], lhsT=BONES, rhs=src[:, b * 512:(b + 1) * 512], start=True, stop=True),
                     reads=['CONST', skey], writes=[f"ps{pb}"])
                res.append(pb)
            return res

        for tb in range(NBLK):
            tsl = slice(tb * TB, (tb + 1) * TB)
            for l, trow in enumerate([T_XW, T_XA, T_XG0, T_XG1, T_VD]):
                if l == 4 and not layer1:
                    continue
                P.dma('sp', PT[0][:], PS[trow * 128:(trow + 1) * 128, tb * TB:tb * TB + TE], reads=psd, writes=["PT0"])
                shift(Ft["T1"], "T1", 0, 6 + l)
                fn = AF.Tanh if l == 0 else (AF.Copy if l in (1, 4) else AF.Sigmoid)
                P.op('act', lambda e: e.activation(out=LO[l][:], in_=Ft["T1"][:], func=fn), reads=["T1"], writes=[f"LO{l}"])
            for j in range(2):
                for i, nn in enumerate(["R", "K", "V"]):
                    trow = T_R0 + 3 * j + i
                    P.dma('sp', PT[i][:], PS[trow * 128:(trow + 1) * 128, tb * TB:tb * TB + TE], reads=psd, writes=[f"PT{i}"])
                    shift(Ft[nn], nn, i, 3 * j + i)
                c = lambda q: COL[:, q, j:j + 1]
                for b in range(2):
                    sl = slice(b * 512, (b + 1) * 512)
                    pw, pa, pg = nps(), nps(), nps()
                    P.op('pe', lambda e: e.matmul(ps[pw][:, :], lhsT=UPB[j][:, 0, :], rhs=LO[0][:, sl], start=True, stop=True), reads=[f"UPB{j}", 'LO0'], writes=[f"ps{pw}"])
                    P.op('pe', lambda e: e.matmul(ps[pa][:, :], lhsT=UPB[j][:, 1, :], rhs=LO[1][:, sl], start=True, stop=True), reads=[f"UPB{j}", 'LO1'], writes=[f"ps{pa}"])
                    P.op('pe', lambda e: e.matmul(ps[pg][:, :], lhsT=UPB[j][:, 2, :], rhs=LO[2][:, sl], start=True, stop=False), reads=[f"UPB{j}", 'LO2'], writes=[f"ps{pg}"])
                    P.op('pe', lambda e: e.matmul(ps[pg][:, :], lhsT=UPB[j][:, 3, :], rhs=LO[3][:, sl], start=False, stop=True), reads=[f"UPB{j}", 'LO3'], writes=[f"ps{pg}"])
                    P.op('act', lambda e: e.activation(out=Ft["E1"][:, sl], in_=ps[pw][:, :], func=AF.Exp, bias=c(0), scale=-1.0), reads=[f"ps{pw}", 'COL'], writes=["E1"])
                    P.op('act', lambda e: e.activation(out=Ft["A"][:, sl], in_=ps[pa][:, :], func=AF.Sigmoid, bias=c(1), scale=1.0), reads=[f"ps{pa}", 'COL'], writes=["A"])
                    P.op('act', lambda e: e.activation(out=Ft["G"][:, sl], in_=ps[pg][:, :], func=AF.Copy), reads=[f"ps{pg}"], writes=["G"])
                    if layer1:
                        pv = nps()
                        P.op('pe', lambda e: e.matmul(ps[pv][:, :], lhsT=UPB[j][:, 4, :], rhs=LO[4][:, sl], start=True, stop=True), reads=[f"UPB{j}", 'LO4'], writes=[f"ps{pv}"])
                        P.op('act', lambda e: e.activation(out=Ft["T2"][:, sl], in_=ps[pv][:, :], func=AF.Sigmoid, bias=c(6), scale=1.0), reads=[f"ps{pv}", 'COL'], writes=["T2"])
                if layer1:
                    P.dma('act', Ft["T1"][:], vfT[j * 128:(j + 1) * 128, tsl], writes=["T1"])
                    P.op('pool', lambda e: e.tensor_tensor(out=Ft["T1"][:], in0=Ft["T1"][:], in1=Ft["V"][:], op=ALU.subtract), reads=["T1", "V"], writes=["T1"])
                    P.op('dve', lambda e: e.tensor_tensor(out=Ft["T1"][:], in0=Ft["T1"][:], in1=Ft["T2"][:], op=ALU.mult), reads=["T1", "T2"], writes=["T1"])
                    P.op('dve', lambda e: e.tensor_tensor(out=Ft["V"][:], in0=Ft["V"][:], in1=Ft["T1"][:], op=ALU.add), reads=["V", "T1"], writes=["V"])
                P.op('dve', lambda e: e.tensor_scalar(out=Ft["E1"][:], in0=Ft["E1"][:], scalar1=1.0, scalar2=None, op0=ALU.add), reads=["E1"], writes=["E1"])
                P.op('dve', lambda e: e.reciprocal(out=Ft["E1"][:], in_=Ft["E1"][:]), reads=["E1"], writes=["E1"])
                P.op('act', lambda e: e.activation(out=Ft["Wd"][:], in_=Ft["E1"][:], func=AF.Exp, scale=-float(np.exp(-0.5))), reads=["E1"], writes=["Wd"])
                P.op('dve', lambda e: e.tensor_scalar(out=Ft["KK"][:], in0=Ft["K"][:], scalar1=c(2), scalar2=None, op0=ALU.mult), reads=["K", 'COL'], writes=["KK"])
                P.op('pool', lambda e: e.tensor_tensor(out=Ft["T1"][:], in0=Ft["KK"][:], in1=Ft["KK"][:], op=ALU.mult), reads=["KK"], writes=["T1"])
                for b, pb in enumerate(bsum(Ft["T1"], "T1")):
                    sl = slice(b * 512, (b + 1) * 512)
                    P.op('dve', lambda e: e.tensor_scalar(out=Ft["T2"][:, sl], in0=ps[pb][:, :], scalar1=1e-18, scalar2=None, op0=ALU.max), reads=[f"ps{pb}"], writes=["T2"])
                P.op('act', lambda e: e.activation(out=Ft["T2"][:], in_=Ft["T2"][:], func=AF.Ln), reads=["T2"], writes=["T2"])
                P.op('act', lambda e: e.activation(out=Ft["T2"][:], in_=Ft["T2"][:], func=AF.Exp, scale=-0.5), reads=["T2"], writes=["T2"])
                P.op('dve', lambda e: e.tensor_tensor(out=Ft["KK"][:], in0=Ft["KK"][:], in1=Ft["T2"][:], op=ALU.mult), reads=["KK", "T2"], writes=["KK"])
                P.op('pool', lambda e: e.tensor_scalar(out=Ft["AA"][:], in0=Ft["KK"][:], scalar1=-1.0, scalar2=None, op0=ALU.mult), reads=["KK"], writes=["AA"])
                P.op('dve', lambda e: e.tensor_tensor(out=Ft["BBv"][:], in0=Ft["KK"][:], in1=Ft["A"][:], op=ALU.mult), reads=["KK", "A"], writes=["BBv"])
                P.op('dve', lambda e: e.tensor_scalar(out=Ft["T1"][:], in0=Ft["A"][:], scalar1=c(3), scalar2=c(4), op0=ALU.mult, op1=ALU.add), reads=["A", 'COL'], writes=["T1"])
                P.op('dve', lambda e: e.tensor_tensor(out=Ft["Kp"][:], in0=Ft["K"][:], in1=Ft["T1"][:], op=ALU.mult), reads=["K", "T1"], writes=["Kp"])
                P.op('pool', lambda e: e.tensor_tensor(out=Ft["T1"][:], in0=Ft["BBv"][:], in1=Ft["R"][:], op=ALU.mult), reads=["BBv", "R"], writes=["T1"])
                P.op('dve', lambda e: e.tensor_tensor(out=Ft["Rp"][:], in0=Ft["Wd"][:], in1=Ft["R"][:], op=ALU.mult), reads=["Wd", "R"], writes=["Rp"])
                for b, pb in enumerate(bsum(Ft["T1"], "T1")):
                    sl = slice(b * 512, (b + 1) * 512)
                    P.op('dve', lambda e: e.tensor_tensor(out=Ft["T2"][:, sl], in0=Ft["AA"][:, sl], in1=ps[pb][:, :], op=ALU.mult), reads=["AA", f"ps{pb}"], writes=["T2"])
                P.op('dve', lambda e: e.tensor_tensor(out=Ft["Rp"][:], in0=Ft["Rp"][:], in1=Ft["T2"][:], op=ALU.add), reads=["Rp", "T2"], writes=["Rp"])
                P.op('pool', lambda e: e.tensor_tensor(out=Ft["T1"][:], in0=Ft["R"][:], in1=Ft["Kp"][:], op=ALU.mult), reads=["R", "Kp"], writes=["T1"])
                for b, pb in enumerate(bsum(Ft["T1"], "T1")):
                    sl = slice(b * 512, (b + 1) * 512)
                    P.op('act', lambda e: e.activation(out=Ft["RK"][:, sl], in_=ps[pb][:, :], func=AF.Copy), reads=[f"ps{pb}"], writes=["RK"])
                P.op('dve', lambda e: e.tensor_scalar(out=Ft["T2"][:], in0=Ft["T1"][:], scalar1=c(5), scalar2=None, op0=ALU.mult), reads=["T1", 'COL'], writes=["T2"])
                for b, pb in enumerate(bsum(Ft["T2"], "T2")):
                    sl = slice(b * 512, (b + 1) * 512)
                    P.op('dve', lambda e: e.tensor_tensor(out=Ft["BON"][:, sl], in0=Ft["V"][:, sl], in1=ps[pb][:, :], op=ALU.mult), reads=["V", f"ps{pb}"], writes=["BON"])
                for sub in range(8):
                    ssl = slice(sub * 128, (sub + 1) * 128)
                    for ai, nn in enumerate(["AA", "BBv", "Kp", "V", "RK"]):
                        pb = nps()
                        P.op('pe', lambda e: e.transpose(out=ps[pb][:, 0:128], in_=Ft[nn][:, ssl], identity=IDENT),
                             reads=[nn, 'CONST'], writes=[f"ps{pb}"])
                        src3 = ps[pb][:, 0:128].rearrange("p (h k) -> p h k", k=64)
                        if nn == "AA":
                            dst, dk = TMA[:, sub, :, :], "TMA"
                        elif nn == "BBv":
                            dst, dk = TMB[:, sub, :, :], "TMB"
                        elif nn == "Kp":
                            dst, dk = TMK[:, sub, :, 0:64], "TMK"
                        elif nn == "V":
                            dst, dk = TMK[:, sub, :, 65:129], "TMK"
                        else:
                            dst, dk, src3 = TMK[:, sub, :, 64:65], "TMK", src3[:, :, 0:1]
                        if ai % 2 == 0:
                            P.op('act', lambda e: e.activation(out=dst, in_=src3, func=AF.Copy), reads=[f"ps{pb}"], writes=[dk])
                        else:
                            P.op('dve', lambda e: e.tensor_copy(out=dst, in_=src3), reads=[f"ps{pb}"], writes=[dk])
                hs = slice(2 * j, 2 * j + 2)
                P.dma('sp', ATM[tsl, hs, :].rearrange("(s p) h k -> p s h k", p=128), TMA[:], reads=["TMA"], writes=["ATMd"])
                P.dma('sp', BTM[tsl, hs, :].rearrange("(s p) h k -> p s h k", p=128), TMB[:], reads=["TMB"], writes=["BTMd"])
                for hh in range(2):
                    P.dma('sp', KRV[2 * j + hh, tsl, :].rearrange("(s p) n -> p s n", p=128), TMK[:, :, hh, :], reads=["TMK"], writes=["KRVd"])
                for hh in range(2):
                    P.dma('pool', RPT[:, 2 * j + hh, tsl], Ft["Rp"][hh * 64:(hh + 1) * 64, :], reads=["Rp"], writes=["RPTd"])
                    P.dma('pool', WTS[:, 2 * j + hh, tsl], Ft["Wd"][hh * 64:(hh + 1) * 64, :], reads=["Wd"], writes=["WTSd"])
                P.dma('pool', BONT[j * 128:(j + 1) * 128, tsl], Ft["BON"][:], reads=["BON"], writes=["BONTd"])
                P.dma('pool', GT[j * 128:(j + 1) * 128, tsl], Ft["G"][:], reads=["G"], writes=["GTd"])
                P.dma('act', vT[j * 128:(j + 1) * 128, tsl], Ft["V"][:], reads=["V"], is_output=True)
    with P.scope():
        S = [[P.sb(f"S{h}_{p}", [97, CH + 1, 64], BF16) for p in range(2)] for h in range(HPC)]
        LT = [[P.sb(f"LT{h}_{p}", [97, CH, 65], BF16) for p in range(2)] for h in range(HPC)]
        A8 = [P.sb(f"A8_{p}", [8, G, HPC, 64], BF16) for p in range(2)]
        BB = [P.sb(f"BB_{p}", [8, G * HPC, 8, 64], BF16) for p in range(2)]
        RP = [P.sb(f"RP_{p}", [64, HPC, CH], F32) for p in range(2)]
        WT = [P.sb(f"WT_{p}", [65, HPC, CH], F32) for p in range(2)]
        psS = [P.ps(f"psS{h}", [128, 512]) for h in range(HPC)]
        psO = [P.ps(f"psO{i}", [128, 512]) for i in range(2)]
        NCH = T // CH
        for p in range(2):
            for h in range(HPC):
                P.op('dve', lambda e: e.memset(S[h][p][:], 0.0), writes=[f"S{h}_{p}"])
                P.op('pool', lambda e: e.memset(LT[h][p][:], 0.0), writes=[f"LT{h}_{p}"])
            P.op('dve', lambda e: e.memset(WT[p][:], 0.0), writes=[f"WT_{p}"])
            P.op('pool', lambda e: e.memset(BB[p][:], 0.0), writes=[f"BB_{p}"])

        def prep(c):
            p = c % 2
            t0 = c * CH
            a8v = ATM[t0:t0 + CH, :, :].rearrange("(g p) h k -> p g h k", p=8)
            b8v = BTM[t0:t0 + CH, :, :].rearrange("(g p) h k -> p g h k", p=8)
            P.dma('sp', A8[p][:], a8v, reads=["ATMd"], writes=[f"A8_{p}"]); yield 'd'
            P.dma('sp', RP[p][:], RPT[:, :, t0:t0 + CH], reads=["RPTd"], writes=[f"RP_{p}"]); yield 'd'
            P.dma('sp', WT[p][0:64, :, :], WTS[:, :, t0:t0 + CH], reads=["WTSd"], writes=[f"WT_{p}"]); yield 'd'
            for j in range(8):
                P.dma('sp', BB[p][j:j + 1, :, j, :].rearrange("p (g h) k -> p g h k", h=HPC), b8v[j:j + 1], reads=["BTMd"], writes=[f"BB_{p}"]); yield 'd'
            for h in range(HPC):
                P.dma('sp', LT[h][p][96:97, :, :], KRV[h:h + 1, t0:t0 + CH, 0:65], reads=["KRVd"], writes=[f"LT{h}_{p}"]); yield 'd'
                P.dma('sp', S[h][p][96:97, 0:CH, :], KRV[h:h + 1, t0:t0 + CH, 65:129], reads=["KRVd"], writes=[f"S{h}_{p}"]); yield 'd'
            for h in range(HPC):
                P.op('pool', lambda e: e.tensor_copy(out=LT[h][p][0:64, :, 64], in_=RP[p][:, h, :]), reads=[f"RP_{p}"], writes=[f"LT{h}_{p}"]); yield 'p'
            for h in range(HPC):
                for g in range(G):
                    o = (h * G + g) % 2
                    P.op('pe', lambda e: e.matmul(psO[o][0:64, 0:512], lhsT=A8[p][:, g, h, :],
                                                  rhs=BB[p][:, g * HPC + h, :, :].rearrange("p j k -> p (j k)"), start=True, stop=True),
                         reads=[f"A8_{p}", f"BB_{p}"], writes=[f"psO{o}"])
                    P.op('act', lambda e: e.activation(out=LT[h][p][0:64, g * 8:(g + 1) * 8, 0:64],
                                                       in_=psO[o][0:64, 0:512].rearrange("p (j k) -> p j k", k=64), func=AF.Copy),
                         reads=[f"psO{o}"], writes=[f"LT{h}_{p}"]); yield 'm'

        def steps(c, gen):
            p = c % 2
            pending = gen is not None
            for tl in range(CH):
                if pending:
                    n_ = 23 if tl == 0 else (-(-(HPC * G) // (CH - CH // 2)) if tl >= CH // 2 else 0)
                    for _ in range(n_):
                        try:
                            next(gen)
                        except StopIteration:
                            pending = False
                            break
                for h in range(HPC):
                    col = (tl % 8) * 64
                    P.op('pe', lambda e: e.matmul(psS[h][0:65, col:col + 64], lhsT=LT[h][p][:, tl, :], rhs=S[h][p][:, tl, :], start=True, stop=True),
                         reads=[f"LT{h}_{p}", f"S{h}_{p}"], writes=[f"psS{h}"])
                    P.op('dve', lambda e: e.scalar_tensor_tensor(out=S[h][p][0:65, tl + 1, :], in0=S[h][p][0:65, tl, :],
                                                                 scalar=WT[p][0:65, h, tl:tl + 1], in1=psS[h][0:65, col:col + 64],
                                                                 op0=ALU.mult, op1=ALU.add),
                         reads=[f"S{h}_{p}", f"WT_{p}", f"psS{h}"], writes=[f"S{h}_{p}"])
            if gen is not None:
                for _ in gen:
                    pass
            t0 = c * CH
            for h in range(HPC):
                P.op('pool', lambda e: e.tensor_copy(out=S[h][1 - p][0:65, 0, :], in_=S[h][p][0:65, CH, :]), reads=[f"S{h}_{p}"], writes=[f"S{h}_{1 - p}"])
                P.dma('pool', YTM[h:h + 1, t0:t0 + CH, :], S[h][p][64:65, 1:CH + 1, :], reads=[f"S{h}_{p}"], writes=["YTMd"])

        for _ in prep(0):
            pass
        for c in range(NCH):
            steps(c, prep(c + 1) if c + 1 < NCH else None)
    with P.scope():
        YT = P.sb("YT", [128, 8, 2, 64], BF16)
        Y = [P.sb(f"Y{i}", [128, TB], F32) for i in range(2)]
        B = [P.sb(f"B{i}", [128, TB], F32) for i in range(2)]
        Gg = [P.sb(f"Gg{i}", [128, TB], F32) for i in range(2)]
        SQ = P.sb("SQ", [128, TB], F32); MU = P.sb("MU", [128, TB], F32); RS = P.sb("RS", [128, TB], F32)
        LNC = P.sb("LNC", [128, 2, 2], F32); EPSG = P.sb("EPSG", [128, 1], F32)
        psT = [P.ps(f"psT{i}", [128, 1024], BF16) for i in range(2)]
        psg = [P.ps(f"psg{i}", [128, 512]) for i in range(4)]
        P.dma('sp', LNC[:], lncols[:, :, :], writes=['LNC'])
        P.op('dve', lambda e: e.memset(EPSG[:], GN_EPS), writes=['EPSG'])
        for tb in range(NBLK):
            tsl = slice(tb * TB, (tb + 1) * TB)
            for j in range(2):
                i = j
                hs = slice(2 * j, 2 * j + 2)
                for hh in range(2):
                    P.dma('sp', YT[:, :, hh, :], YTM[2 * j + hh, tsl, :].rearrange("(s p) k -> p s k", p=128), reads=["YTMd"], writes=["YT"])
                P.dma('act', B[i][:], BONT[j * 128:(j + 1) * 128, tsl], reads=["BONTd"], writes=[f"B{i}"])
                P.dma('sp', Gg[i][:], GT[j * 128:(j + 1) * 128, tsl], reads=["GTd"], writes=[f"Gg{i}"])
                for sub in range(8):
                    P.op('pe', lambda e: e.transpose(out=psT[i][:, sub * 128:(sub + 1) * 128], in_=YT[:, sub, :, :].rearrange("p h k -> p (h k)"), identity=IDB[:]),
                         reads=["YT", 'IDB'], writes=[f"psT{i}"])
                P.op('act', lambda e: e.activation(out=Y[i][:], in_=psT[i][:, :], func=AF.Copy), reads=[f"psT{i}"], writes=[f"Y{i}"])
                P.op('act', lambda e: e.activation(out=SQ[:], in_=Y[i][:], func=AF.Square), reads=[f"Y{i}"], writes=['SQ'])
                for b in range(2):
                    sl = slice(b * 512, (b + 1) * 512)
                    pm, pv = 2 * b, 2 * b + 1
                    P.op('pe', lambda e: e.matmul(psg[pm][:, :], lhsT=BONES64, rhs=Y[i][:, sl], start=True, stop=True), reads=['CONST', f"Y{i}"], writes=[f"psg{pm}"])
                    P.op('pe', lambda e: e.matmul(psg[pv][:, :], lhsT=BONES64, rhs=SQ[:, sl], start=True, stop=True), reads=['CONST', 'SQ'], writes=[f"psg{pv}"])
                    P.op('act', lambda e: e.activation(out=MU[:, sl], in_=psg[pm][:, :], func=AF.Copy), reads=[f"psg{pm}"], writes=['MU'])
                    P.op('dve', lambda e: e.tensor_tensor(out=RS[:, sl], in0=MU[:, sl], in1=MU[:, sl], op=ALU.mult), reads=['MU'], writes=['RS'])
                    P.op('dve', lambda e: e.tensor_tensor(out=RS[:, sl], in0=psg[pv][:, :], in1=RS[:, sl], op=ALU.subtract), reads=[f"psg{pv}", 'RS'], writes=['RS'])
                P.op('act', lambda e: e.activation(out=RS[:], in_=RS[:], func=AF.Ln, bias=EPSG[:, 0:1], scale=1.0), reads=['RS', 'EPSG'], writes=['RS'])
                P.op('act', lambda e: e.activation(out=RS[:], in_=RS[:], func=AF.Exp, scale=-0.5), reads=['RS'], writes=['RS'])
                P.op('dve', lambda e: e.tensor_tensor(out=Y[i][:], in0=Y[i][:], in1=MU[:], op=ALU.subtract), reads=[f"Y{i}", 'MU'], writes=[f"Y{i}"])
                P.op('dve', lambda e: e.tensor_tensor(out=Y[i][:], in0=Y[i][:], in1=RS[:], op=ALU.mult), reads=[f"Y{i}", 'RS'], writes=[f"Y{i}"])
                P.op('dve', lambda e: e.tensor_scalar(out=Y[i][:], in0=Y[i][:], scalar1=LNC[:, 0, j:j + 1], scalar2=LNC[:, 1, j:j + 1],
                                                      op0=ALU.mult, op1=ALU.add), reads=[f"Y{i}", 'LNC'], writes=[f"Y{i}"])
                P.op('pool', lambda e: e.tensor_tensor(out=Y[i][:], in0=Y[i][:], in1=B[i][:], op=ALU.add), reads=[f"Y{i}", f"B{i}"], writes=[f"Y{i}"])
                P.op('pool', lambda e: e.tensor_tensor(out=Y[i][:], in0=Y[i][:], in1=Gg[i][:], op=ALU.mult), reads=[f"Y{i}", f"Gg{i}"], writes=[f"Y{i}"])
                P.dma('pool', yrwT[j * 128:(j + 1) * 128, tsl], Y[i][:], reads=[f"Y{i}"], is_output=True)
    P.finish('sp')
    print("L1 ninstr", P.ninstr, "nsem", P.nsem)
    es.close()
    return nc

POOL_WINDOWS = (2, 4, 8, 16)

def l1_consts():
    bo = np.zeros((128, 128), np.float32); bo[:64, :64] = 1; bo[64:, 64:] = 1
    return np.stack([np.ones((128, 128), np.float32), bo, bo / 64, np.eye(128, dtype=np.float32)], 0)

def l1_core_inputs(c, xT, L, w):
    f32 = np.float32
    g, half = c // 2, c % 2
    W = w['w_in'][L]
    base = 3072
    chs = [256 * c + j * 128 + np.arange(128) for j in range(2)]
    colsets = [g * 256 + np.arange(128), g * 256 + 128 + np.arange(128),
               1024 + c * 128 + np.arange(128), 2048 + c * 128 + np.arange(128)]
    tiles = [np.ascontiguousarray(W[:, cs]) for cs in colsets]
    def padcols(a):
        return np.concatenate([a, np.zeros((a.shape[0], 128 - a.shape[1]), f32)], 1)
    tiles.append(padcols(W[:, base + 6144:base + 6240])); tiles.append(padcols(W[:, base + 6240:base + 6336]))
    tiles.append(W[:, base + 6336:base + 6464]); tiles.append(W[:, base + 6464:base + 6592])
    tiles.append(padcols(w['rwkv_v_down'][L - 1]) if L > 0 else np.zeros((4096, 128), f32))
    for j in range(2):
        for off in (0, 2048, 4096):
            tiles.append(W[:, base + off + chs[j]])
    wt = np.stack([t.reshape(32, 128, 128).transpose(1, 0, 2) for t in tiles], 0)
    sm = w['shift_mix'][L]
    mixc = np.zeros((128, 11), f32)
    for j in range(2):
        for i, off in enumerate((0, 2048, 4096)):
            mixc[:, 3 * j + i] = sm[off + chs[j]]
    mixc[:96, 6] = sm[6144:6240]; mixc[:96, 7] = sm[6240:6336]; mixc[:, 8] = sm[6336:6464]; mixc[:, 9] = sm[6464:6592]
    if L > 0:
        mixc[:64, 10] = w['rwkv_v_shift'][L - 1]
    cols = np.zeros((128, 8, 2), f32)
    rk = w['rwkv_r_k'][L].reshape(-1)
    for j in range(2):
        ch = chs[j]
        cols[:, 0, j] = w['rwkv_w0'][L][ch]; cols[:, 1, j] = w['rwkv_a0'][L][ch]; cols[:, 2, j] = w['rwkv_k_k'][L][ch]
        cols[:, 3, j] = w['rwkv_k_a'][L][ch]; cols[:, 5, j] = rk[ch]
        if L > 0:
            cols[:, 6, j] = w['rwkv_v0'][L - 1][ch]
    ups = np.zeros((2, 128, 5, 128), f32)
    for j in range(2):
        ch = chs[j]
        ups[j, :96, 0] = w['rwkv_w_up'][L][:, ch]; ups[j, :96, 1] = w['rwkv_a_up'][L][:, ch]
        ups[j, :, 2] = w['rwkv_g_up'][L][:128, ch]; ups[j, :, 3] = w['rwkv_g_up'][L][128:, ch]
        if L > 0:
            ups[j, :64, 4] = w['rwkv_v_up'][L - 1][:, ch]
    lncols = np.stack([np.stack([w['rwkv_ln_g'][L][chs[j]] for j in range(2)], 1),
                       np.stack([w['rwkv_ln_b'][L][chs[j]] for j in range(2)], 1)], 1)
    pw = w['pool_w'][L][g]
    pwt = np.ascontiguousarray(pw[:, half * 128:(half + 1) * 128].reshape(2, 128, 128).transpose(1, 0, 2))
    pcol = np.zeros((128, 5), f32); pcol[:, g] = 1.0 / POOL_WINDOWS[g]
    pcol[:, 4] = w['pool_scale'][L][g * 256 + half * 128 + np.arange(128)]
    pinv = np.zeros((128, 4, 16), f32); pinv[:, g, :] = 1.0 / np.minimum(np.arange(16) + 1, POOL_WINDOWS[g])
    dwT = np.zeros((128, KW + 1), f32)
    dwT[:, :KW] = w['conv_dw'][L][:, c * 128:(c + 1) * 128].T; dwT[:, KW] = w['conv_dw_b'][L][c * 128:(c + 1) * 128]
    m = {"xT": xT, "gain": np.ascontiguousarray(w['mix_norm'][L].reshape(32, 128).T), "wt": wt, "consts": l1_consts(),
         "pwt": pwt, "pcol": pcol, "pinv": pinv, "dwT": dwT, "mixc": mixc, "cols": cols, "ups": ups, "lncols": lncols}
    return {k: np.ascontiguousarray(v, dtype=f32) for k, v in m.items()}

I32 = mybir.dt.int32
AX = mybir.AxisListType.X
NE = 64
EPS = 1e-6
LN_EPS = 1e-5


def n_moe_blocks(T):
    return -(-(2 * T + NE * 127) // 128)


def build_l3(T, final, mode='full'):
    nc = bass.Bass("TRN2", target_bir_lowering=False)
    NBLK = T // TB
    NT = T // 128
    NB = n_moe_blocks(T)
    din = lambda n, s, dt=F32: nc.dram_tensor(n, s, dt, kind="ExternalInput").ap()
    dscr = lambda n, s, dt=F32: nc.dram_tensor(n, s, dt).ap()
    FRONT, BACK = mode in ('full', 'front'), mode in ('full', 'back')
    dout = lambda n, s, dt=F32: nc.dram_tensor(n, s, dt, kind="ExternalOutput").ap()
    consts = din("consts", [3, 128, 128])
    grows = din("grows", [2, D])
    if FRONT:
        xT = din("xT", [D, T]); mixT = din("mixT", [D, T])
        cvcols = din("cvcols", [128, 2, 8]); pwt = din("pwt", [8, 128, 8, 128])
        wot = din("wot", [KC, 128, KC, 128])
        fgain = din("fgain", [128, KC])
        wr = din("wr", [128, KC, 72]); rbias = din("rbias", [1, 72])
        YCONV = dscr("YCONV", [1024, T]); X1T = dscr("X1T", [D, T])
    if BACK:
        ewin = din("ewin", [NE * 128 * 4, 8192]); ewout = din("ewout", [NE * 128 * 2, 8192])
        bgrid = din("bgrid", [64, NB]); iotap = din("iotap", [128, 1])
        tokrow = din("tokrow", [128, NT, 16], I32)
        out = dout("out", [T, D])
    if mode == 'full':
        X1 = dscr("X1", [T, D]); HN = dscr("HN", [T + 128, D])
    elif mode == 'front':
        X1 = dout("x1_out", [T, D]); HN = dout("hn_out", [T, D])
        mk_out = dout("mk_out", [128, NT, 2, 64]); gates_out = dout("gates_out", [128, NT, 2])
    else:
        X1 = din("x1_in", [T, D]); HN = din("hn_in", [T + 128, D])
        mk_in = din("mk_in", [128, NT, 2, 64]); gates_in = din("gates_in", [128, NT, 2])
    if BACK:
        SLOTTOK = dscr("SLOTTOK", [NB * 128, 16], I32); YS = dscr("YS", [NB * 128, D], BF16)
    es = ExitStack(); P = Prog(nc, es)
    CONST = P.sb("CONST", [128, 3, 128], F32)
    P.dma('sp', CONST[:], consts.rearrange("c p n -> p c n"), writes=['CONST'])
    ONES, IDENT, TRI = CONST[:, 0, :], CONST[:, 1, :], CONST[:, 2, :]
    GATES = P.sb("GATES", [128, NT, 2], F32)
    DESTI = P.sb("DESTI", [128, NT, 2], I32)
    WINI = P.sb("WINI", [128, NB, 4], I32); WOUTI = P.sb("WOUTI", [128, NB, 2], I32)
    GBC = P.sb("GBC", [128, D], F32)

    def bcast_row(dst, dkey, row_ap, width, psl, rowt):
        P.dma('sp', rowt[0:1, 0:width], row_ap, writes=['ROWT'])
        for b in range(0, width, 512):
            wdt = min(512, width - b)
            P.op('pe', lambda e: e.matmul(psl[:, 0:wdt], lhsT=ONES[0:1, :], rhs=rowt[0:1, b:b + wdt], start=True, stop=True),
                 reads=['CONST', 'ROWT'], writes=['psl'])
            P.op('act', lambda e: e.activation(out=dst[:, b:b + wdt], in_=psl[:, 0:wdt], func=AF.Copy), reads=['psl'], writes=[dkey])

    if FRONT:
        with P.scope():
            HC = [P.sb(f"HC{j}", [128, TB], F32) for j in range(8)]
            SQ = [P.sb(f"SQ{i}", [128, TB], F32) for i in range(2)]
            MEAN = P.sb("MEAN", [128, TB], F32); RSTD = P.sb("RSTD", [128, TB], F32); M2 = P.sb("M2", [128, TB], F32)
            AC = [P.sb(f"AC{j}", [128, TB], BF16) for j in range(8)]
            PWF = [P.sb(f"PWF{i}", [128, 8, 128], F32) for i in range(2)]
            PWB = [P.sb(f"PWB{i}", [128, 8, 128], BF16) for i in range(2)]
            OT = [P.sb(f"OT{i}", [128, TB], F32) for i in range(2)]
            COL = P.sb("COL", [128, 2, 8], F32); EPSC = P.sb("EPSC", [128, 1], F32)
            psM = [P.ps(f"psM{b}", [128, 512]) for b in range(2)]
            psV = [P.ps(f"psV{b}", [128, 512]) for b in range(2)]
            psO = [P.ps(f"psO{i}", [128, 512]) for i in range(2)]
            P.dma('sp', COL[:], cvcols[:, :, :], writes=['COL'])
            P.op('dve', lambda e: e.memset(EPSC[:], LN_EPS), writes=['EPSC'])
            for tb in range(NBLK):
                tsl = slice(tb * TB, (tb + 1) * TB)
                for j in range(8):
                    i = j % 2
                    P.dma('sp' if i == 0 else 'act', HC[j][:], mixT[1024 + j * 128:1024 + (j + 1) * 128, tsl], writes=[f"HC{j}"])
                    P.op('act', lambda e: e.activation(out=SQ[i][:], in_=HC[j][:], func=AF.Square), reads=[f"HC{j}"], writes=[f"SQ{i}"])
                    for b in range(2):
                        sl = slice(b * 512, (b + 1) * 512)
                        P.op('pe', lambda e: e.matmul(psM[b][:, :], lhsT=ONES, rhs=HC[j][:, sl], start=(j == 0), stop=(j == 7)), reads=['CONST', f"HC{j}"], writes=[f"psM{b}"])
                        P.op('pe', lambda e: e.matmul(psV[b][:, :], lhsT=ONES, rhs=SQ[i][:, sl], start=(j == 0), stop=(j == 7)), reads=['CONST', f"SQ{i}"], writes=[f"psV{b}"])
                for b in range(2):
                    sl = slice(b * 512, (b + 1) * 512)
                    P.op('act', lambda e: e.activation(out=MEAN[:, sl], in_=psM[b][:, :], func=AF.Copy, scale=1.0 / 1024), reads=[f"psM{b}"], writes=['MEAN'])
                    P.op('dve', lambda e: e.tensor_tensor(out=M2[:, sl], in0=MEAN[:, sl], in1=MEAN[:, sl], op=ALU.mult), reads=['MEAN'], writes=['M2'])
                    P.op('dve', lambda e: e.scalar_tensor_tensor(out=RSTD[:, sl], in0=psV[b][:, :], scalar=1.0 / 1024, in1=M2[:, sl],
                                                                 op0=ALU.mult, op1=ALU.subtract), reads=[f"psV{b}", 'M2'], writes=['RSTD'])
                P.op('act', lambda e: e.activation(out=RSTD[:], in_=RSTD[:], func=AF.Ln, bias=EPSC[:, 0:1], scale=1.0), reads=['RSTD', 'EPSC'], writes=['RSTD'])
                P.op('act', lambda e: e.activation(out=RSTD[:], in_=RSTD[:], func=AF.Exp, scale=-0.5), reads=['RSTD'], writes=['RSTD'])
                for j in range(8):
                    eng = 'dve' if j % 2 == 0 else 'pool'
                    P.op(eng, lambda e: e.tensor_tensor(out=HC[j][:], in0=HC[j][:], in1=MEAN[:], op=ALU.subtract), reads=[f"HC{j}", 'MEAN'], writes=[f"HC{j}"])
                    P.op(eng, lambda e: e.tensor_tensor(out=HC[j][:], in0=HC[j][:], in1=RSTD[:], op=ALU.mult), reads=[f"HC{j}", 'RSTD'], writes=[f"HC{j}"])
                    P.op('act', lambda e: e.activation(out=AC[j][:], in_=HC[j][:], func=AF.Silu, bias=COL[:, 1, j:j + 1], scale=COL[:, 0, j:j + 1]),
                         reads=[f"HC{j}", 'COL'], writes=[f"AC{j}"])
                ack = [f"AC{j}" for j in range(8)]
                for dt in range(8):
                    i = dt % 2
                    P.dma('sp', PWF[i][:], pwt[dt], writes=[f"PWF{i}"])
                    P.op('pool', lambda e: e.tensor_copy(out=PWB[i][:], in_=PWF[i][:]), reads=[f"PWF{i}"], writes=[f"PWB{i}"])
                    for b in range(2):
                        pb = b
                        for kc in range(8):
                            P.op('pe', lambda e: e.matmul(psO[pb][:, :], lhsT=PWB[i][:, kc, :], rhs=AC[kc][:, b * 512:(b + 1) * 512], start=(kc == 0), stop=(kc == 7)),
                                 reads=[f"PWB{i}"] + (ack if dt == 0 and b == 0 else []), writes=[f"psO{pb}"])
                        P.op('act', lambda e: e.activation(out=OT[i][:, b * 512:(b + 1) * 512], in_=psO[pb][:, :], func=AF.Copy), reads=[f"psO{pb}"], writes=[f"OT{i}"])
                    P._commit((P.cur['pe'][0], P.cur['pe'][1]), ack + [f"PWB{i}"], [])
                    P.dma('pool', YCONV[dt * 128:(dt + 1) * 128, tsl], OT[i][:], reads=[f"OT{i}"], writes=["YCd"])
        with P.scope():
            MB = P.sb("MB", [128, KC, TB], BF16)
            MF = [P.sb(f"MF{i}", [128, TB], F32) for i in range(2)]
            WF = [P.sb(f"WF{i}", [128, KC, 128], F32) for i in range(2)]
            WB = [P.sb(f"WB{i}", [128, KC, 128], BF16) for i in range(2)]
            XI = [P.sb(f"XI{i}", [128, TB], F32) for i in range(2)]
            psB = [P.ps(f"psB{i}", [128, 512]) for i in range(4)]
            for tb in range(NBLK):
                tsl = slice(tb * TB, (tb + 1) * TB)
                for kc in range(KC):
                    i = kc % 2
                    src = YCONV[(kc - 8) * 128:(kc - 7) * 128, tsl] if 8 <= kc < 16 else mixT[kc * 128:(kc + 1) * 128, tsl]
                    P.dma('sp' if i == 0 else 'act', MF[i][:], src, writes=[f"MF{i}"])
                    if kc % 3 == 0:
                        P.op('dve', lambda e: e.tensor_copy(out=MB[:, kc, :], in_=MF[i][:]), reads=[f"MF{i}"], writes=[f"MB{kc}"])
                    elif kc % 3 == 1:
                        P.op('pool', lambda e: e.tensor_copy(out=MB[:, kc, :], in_=MF[i][:]), reads=[f"MF{i}"], writes=[f"MB{kc}"])
                    else:
                        P.op('act', lambda e: e.activation(out=MB[:, kc, :], in_=MF[i][:], func=AF.Copy), reads=[f"MF{i}"], writes=[f"MB{kc}"])
                mbk = [f"MB{kc}" for kc in range(KC)]
                for dt in range(KC):
                    i = dt % 2
                    P.dma('sp', WF[i][:], wot[dt], writes=[f"WF{i}"])
                    P.op('pool', lambda e: e.tensor_copy(out=WB[i][:], in_=WF[i][:]), reads=[f"WF{i}"], writes=[f"WB{i}"])
                    P.dma('act', XI[i][:], xT[dt * 128:(dt + 1) * 128, tsl], writes=[f"XI{i}"])
                    for b in range(2):
                        pb = (dt * 2 + b) % 4
                        for kc in range(KC):
                            P.op('pe', lambda e: e.matmul(psB[pb][:, :], lhsT=WB[i][:, kc, :], rhs=MB[:, kc, b * 512:(b + 1) * 512], start=(kc == 0), stop=(kc == KC - 1)),
                                 reads=([f"WB{i}"] + (mbk if dt == 0 and b == 0 else [])) if kc == 0 else [], writes=[f"psB{pb}"])
                        P.op('dve', lambda e: e.tensor_tensor(out=XI[i][:, b * 512:(b + 1) * 512], in0=psB[pb][:, :], in1=XI[i][:, b * 512:(b + 1) * 512], op=ALU.add),
                             reads=[f"psB{pb}", f"XI{i}"], writes=[f"XI{i}"])
                    P._commit((P.cur['pe'][0], P.cur['pe'][1]), mbk + [f"WB{i}"], [])
                    P.dma('pool', X1T[dt * 128:(dt + 1) * 128, tsl], XI[i][:], reads=[f"XI{i}"], writes=["X1Td"])
    MKs = ExitStack()
    P.es = MKs
    MK = P.sb("MK", [128, NT, 2, 64], F32)
    RSEL = P.sb("RSEL", [128, NT, 2], F32)
    CARRY = P.sb("CARRY", [1, 64], F32)
    P.es = es
    if FRONT:
        with P.scope():
            X1C1 = P.sb("X1C0", [128, KC, 128], F32); X1C = [X1C1, X1C1]
            X1G = P.sb("X1G", [128, KC, 128], F32)
            X1R = [P.sb(f"X1R{i}", [128, D], F32) for i in range(2)]
            HNt1 = P.sb("HNt0", [128, D], F32); HNt = [HNt1, HNt1]
            JUNK = P.sb("JUNK", [128, D], BF16)
            FG = P.sb("FG", [128, KC], F32); WR = P.sb("WR", [128, KC, 72], F32); RBC = P.sb("RBC", [128, 72], F32)
            ROWT = P.sb("ROWT", [1, D], F32)
            SM = {n: P.sb("sm_" + n, [128, w_], F32) for n, w_ in
                  [("SS", 1), ("RS", 1), ("LG", 72), ("GMAX", 1), ("NG", 1), ("OHG", 8), ("EXG", 8), ("SE", 1), ("PG", 1), ("SEL", 8), ("M1", 1),
                   ("OH1", 8), ("SEL2", 8), ("M2", 1), ("OH2", 8), ("DD", 1), ("ED", 1), ("RDEN", 1), ("MSUM", 64), ("T64", 64), ("EPS", 1)]}
            psl = P.ps("psl", [128, 512]); psR = P.ps("psR", [128, 512]); psK = P.ps("psK", [128, 512]); psC = P.ps("psC", [128, 512])
            psT = [P.ps(f"psT{i}", [128, 512]) for i in range(2)]
            P.dma('sp', FG[:], fgain[:, :], writes=['FG']); P.dma('sp', WR[:], wr[:, :, :], writes=['WR'])
            bcast_row(GBC, 'GBC', grows[0:1, :], D, psl, ROWT)
            bcast_row(RBC, 'RBC', rbias[0:1, :], 72, psl, ROWT)
            P.op('dve', lambda e: e.memset(SM["EPS"][:], EPS), writes=['EPS'])
            P.op('dve', lambda e: e.memset(CARRY[:], 0.0), writes=['CARRY'])
            if mode == 'full':
                P.op('pool', lambda e: e.memset(HNt[1][:], 0.0), writes=["HNt0"])
                P.dma('sp', HN[T:T + 128, :], HNt[1][:], reads=["HNt0"], writes=["HNd"])
            s = lambda n: SM[n]
            for tt in range(NT):
                i = tt % 2
                tsl = slice(tt * 128, (tt + 1) * 128)
                P.dma('sp', X1C[i][:], X1T[:, tsl].rearrange("(kc p) t -> p kc t", p=128), reads=["X1Td"], writes=["X1C0"])
                for kc in range(KC):
                    P.op('dve' if kc % 2 == 0 else 'pool',
                         lambda e: e.tensor_scalar(out=X1G[:, kc, :], in0=X1C[i][:, kc, :], scalar1=FG[:, kc:kc + 1], scalar2=None, op0=ALU.mult),
                         reads=["X1C0", 'FG'], writes=[f"X1G{kc}"])
                for kc in range(KC):
                    P.op('pe', lambda e: e.matmul(psR[:, 0:72], lhsT=X1G[:, kc, :], rhs=WR[:, kc, :], start=(kc == 0), stop=(kc == KC - 1)),
                         reads=[f"X1G{kc}", 'WR'], writes=['psR'])
                for k4 in range(8):
                    pt = psT[k4 % 2]
                    for q in range(4):
                        kc = k4 * 4 + q
                        P.op('pe', lambda e: e.transpose(out=pt[:, q * 128:(q + 1) * 128], in_=X1C[i][:, kc, :], identity=IDENT),
                             reads=["X1C0", 'CONST'], writes=[f"psT{k4 % 2}"])
                    if k4 % 2 == 0:
                        P.op('act', lambda e: e.activation(out=X1R[i][:, k4 * 512:(k4 + 1) * 512], in_=pt[:, :], func=AF.Copy), reads=[f"psT{k4 % 2}"], writes=[f"X1R{i}"])
                    else:
                        P.op('dve', lambda e: e.tensor_copy(out=X1R[i][:, k4 * 512:(k4 + 1) * 512], in_=pt[:, :]), reads=[f"psT{k4 % 2}"], writes=[f"X1R{i}"])
                P.op('act', lambda e: e.activation(out=JUNK[:], in_=X1R[i][:], func=AF.Square, accum_out=s("SS")[:, 0:1]), reads=[f"X1R{i}"], writes=['JUNK', 'SS'])
                P.op('act', lambda e: e.activation(out=s("RS")[:], in_=s("SS")[:], func=AF.Ln, bias=s("EPS")[:, 0:1], scale=1.0 / D), reads=['SS', 'EPS'], writes=['RS'])
                P.op('act', lambda e: e.activation(out=s("RS")[:], in_=s("RS")[:], func=AF.Exp, scale=-0.5), reads=['RS'], writes=['RS'])
                P.op('dve', lambda e: e.scalar_tensor_tensor(out=HNt[i][:], in0=X1R[i][:], scalar=s("RS")[:, 0:1], in1=GBC[:], op0=ALU.mult, op1=ALU.mult),
                     reads=[f"X1R{i}", 'RS', 'GBC'], writes=["HNt0"])
                P.dma('pool', HN[tsl, :], HNt[i][:], reads=["HNt0"], writes=["HNd"], is_output=(mode == 'front'))
                P.dma('act', X1[tsl, :], X1R[i][:], reads=[f"X1R{i}"], writes=["X1d"], is_output=(mode == 'front'))
                P.op('dve', lambda e: e.scalar_tensor_tensor(out=s("LG")[:], in0=psR[:, 0:72], scalar=s("RS")[:, 0:1], in1=RBC[:], op0=ALU.mult, op1=ALU.add),
                     reads=['psR', 'RS', 'RBC'], writes=['LG'])
                LG = s("LG")
                P.op('dve', lambda e: e.reduce_max(out=s("GMAX")[:], in_=LG[:, 0:8], axis=AX), reads=['LG'], writes=['GMAX'])
                P.op('dve', lambda e: e.tensor_scalar(out=s("OHG")[:], in0=LG[:, 0:8], scalar1=s("GMAX")[:, 0:1], scalar2=None, op0=ALU.is_equal), reads=['LG', 'GMAX'], writes=['OHG'])
                P.op('dve', lambda e: e.tensor_scalar(out=s("NG")[:], in0=s("GMAX")[:], scalar1=-1.0, scalar2=None, op0=ALU.mult), reads=['GMAX'], writes=['NG'])
                P.op('act', lambda e: e.activation(out=s("EXG")[:], in_=LG[:, 0:8], func=AF.Exp, bias=s("NG")[:, 0:1], scale=1.0, accum_out=s("SE")[:, 0:1]),
                     reads=['LG', 'NG'], writes=['EXG', 'SE'])
                P.op('dve', lambda e: e.reciprocal(out=s("PG")[:], in_=s("SE")[:]), reads=['SE'], writes=['PG'])
                P.op('dve', lambda e: e.tensor_scalar(out=s("SEL")[:], in0=LG[:, 8:16], scalar1=s("OHG")[:, 0:1], scalar2=None, op0=ALU.mult), reads=['LG', 'OHG'], writes=['SEL'])
                for g in range(1, 8):
                    P.op('dve', lambda e: e.scalar_tensor_tensor(out=s("SEL")[:], in0=LG[:, 8 + 8 * g:16 + 8 * g], scalar=s("OHG")[:, g:g + 1], in1=s("SEL")[:],
                                                                 op0=ALU.mult, op1=ALU.add), reads=['LG', 'OHG', 'SEL'], writes=['SEL'])
                P.op('dve', lambda e: e.reduce_max(out=s("M1")[:], in_=s("SEL")[:], axis=AX), reads=['SEL'], writes=['M1'])
                P.op('dve', lambda e: e.tensor_scalar(out=s("OH1")[:], in0=s("SEL")[:], scalar1=s("M1")[:, 0:1], scalar2=None, op0=ALU.is_equal), reads=['SEL', 'M1'], writes=['OH1'])
                P.op('dve', lambda e: e.scalar_tensor_tensor(out=s("SEL2")[:], in0=s("OH1")[:], scalar=-1e30, in1=s("SEL")[:], op0=ALU.mult, op1=ALU.add),
                     reads=['OH1', 'SEL'], writes=['SEL2'])
                P.op('dve', lambda e: e.reduce_max(out=s("M2")[:], in_=s("SEL2")[:], axis=AX), reads=['SEL2'], writes=['M2'])
                P.op('dve', lambda e: e.tensor_scalar(out=s("OH2")[:], in0=s("SEL2")[:], scalar1=s("M2")[:, 0:1], scalar2=None, op0=ALU.is_equal), reads=['SEL2', 'M2'], writes=['OH2'])
                P.op('dve', lambda e: e.tensor_tensor(out=s("DD")[:], in0=s("M2")[:], in1=s("M1")[:], op=ALU.subtract), reads=['M1', 'M2'], writes=['DD'])
                P.op('act', lambda e: e.activation(out=s("ED")[:], in_=s("DD")[:], func=AF.Exp), reads=['DD'], writes=['ED'])
                P.op('dve', lambda e: e.tensor_scalar(out=s("ED")[:], in0=s("ED")[:], scalar1=1.0, scalar2=None, op0=ALU.add), reads=['ED'], writes=['ED'])
                P.op('dve', lambda e: e.reciprocal(out=s("RDEN")[:], in_=s("ED")[:]), reads=['ED'], writes=['RDEN'])
                P.op('dve', lambda e: e.tensor_tensor(out=GATES[:, tt, 0:1], in0=s("PG")[:], in1=s("RDEN")[:], op=ALU.mult), reads=['PG', 'RDEN'], writes=['GATES'])
                P.op('dve', lambda e: e.tensor_tensor(out=GATES[:, tt, 1:2], in0=s("PG")[:], in1=GATES[:, tt, 0:1], op=ALU.subtract), reads=['PG', 'GATES'], writes=['GATES'])
                for g in range(8):
                    P.op('pool', lambda e: e.tensor_scalar(out=MK[:, tt, 0, g * 8:(g + 1) * 8], in0=s("OH1")[:], scalar1=s("OHG")[:, g:g + 1], scalar2=None, op0=ALU.mult),
                         reads=['OH1', 'OHG'], writes=['MK'])
                    P.op('pool', lambda e: e.tensor_scalar(out=MK[:, tt, 1, g * 8:(g + 1) * 8], in0=s("OH2")[:], scalar1=s("OHG")[:, g:g + 1], scalar2=None, op0=ALU.mult),
                         reads=['OH2', 'OHG'], writes=['MK'])
                P.op('dve', lambda e: e.tensor_tensor(out=s("MSUM")[:], in0=MK[:, tt, 0, :], in1=MK[:, tt, 1, :], op=ALU.add), reads=['MK'], writes=['MSUM'])
                P.op('pe', lambda e: e.matmul(psK[:, 0:64], lhsT=TRI, rhs=s("MSUM")[:], start=True, stop=False), reads=['CONST', 'MSUM'], writes=['psK'])
                P.op('pe', lambda e: e.matmul(psK[:, 0:64], lhsT=ONES[0:1, :], rhs=CARRY[0:1, :], start=False, stop=True), reads=['CONST', 'CARRY'], writes=['psK'])
                for k in range(2):
                    P.op('dve', lambda e: e.tensor_tensor(out=s("T64")[:], in0=MK[:, tt, k, :], in1=psK[:, 0:64], op=ALU.mult), reads=['MK', 'psK'], writes=['T64'])
                    P.op('dve', lambda e: e.reduce_sum(out=RSEL[:, tt, k:k + 1], in_=s("T64")[:], axis=AX), reads=['T64'], writes=['RSEL'])
                P.op('pe', lambda e: e.matmul(psC[0:1, 0:64], lhsT=ONES[:, 0:1], rhs=s("MSUM")[:], start=True, stop=True), reads=['CONST', 'MSUM'], writes=['psC'])
                P.op('dve', lambda e: e.tensor_tensor(out=CARRY[0:1, :], in0=CARRY[0:1, :], in1=psC[0:1, 0:64], op=ALU.add), reads=['CARRY', 'psC'], writes=['CARRY'])
        if mode == 'front':
            P.dma('sp', mk_out[:, :, :, :], MK[:], reads=['MK'], is_output=True)
            P.dma('sp', gates_out[:, :, :], GATES[:], reads=['GATES'], is_output=True)
    else:
        P.dma('sp', MK[:], mk_in[:, :, :, :], writes=['MK'])
        P.dma('sp', GATES[:], gates_in[:, :, :], writes=['GATES'])
        with P.scope():
            MSUM = P.sb("MSUMb", [128, 64], F32); T64r = P.sb("T64r", [128, 64], F32)
            psK = P.ps("psKb", [128, 512]); psC = P.ps("psCb", [128, 512])
            P.op('dve', lambda e: e.memset(CARRY[:], 0.0), writes=['CARRY'])
            for tt in range(NT):
                P.op('dve', lambda e: e.tensor_tensor(out=MSUM[:], in0=MK[:, tt, 0, :], in1=MK[:, tt, 1, :], op=ALU.add), reads=['MK'], writes=['MSUM'])
                P.op('pe', lambda e: e.matmul(psK[:, 0:64], lhsT=TRI, rhs=MSUM[:], start=True, stop=False), reads=['CONST', 'MSUM'], writes=['psK'])
                P.op('pe', lambda e: e.matmul(psK[:, 0:64], lhsT=ONES[0:1, :], rhs=CARRY[0:1, :], start=False, stop=True), reads=['CONST', 'CARRY'], writes=['psK'])
                for k in range(2):
                    P.op('dve', lambda e: e.tensor_tensor(out=T64r[:], in0=MK[:, tt, k, :], in1=psK[:, 0:64], op=ALU.mult), reads=['MK', 'psK'], writes=['T64'])
                    P.op('dve', lambda e: e.reduce_sum(out=RSEL[:, tt, k:k + 1], in_=T64r[:], axis=AX), reads=['T64'], writes=['RSEL'])
                P.op('pe', lambda e: e.matmul(psC[0:1, 0:64], lhsT=ONES[:, 0:1], rhs=MSUM[:], start=True, stop=True), reads=['CONST', 'MSUM'], writes=['psC'])
                P.op('dve', lambda e: e.tensor_tensor(out=CARRY[0:1, :], in0=CARRY[0:1, :], in1=psC[0:1, 0:64], op=ALU.add), reads=['CARRY', 'psC'], writes=['CARRY'])
    if BACK:
        with P.scope():
            CI = P.sb("CI", [1, 64], I32); PAD = P.sb("PAD", [1, 64], F32)
            PE_ = [P.sb(f"PE{i}", [1, 64], F32) for i in range(2)]
            PST = P.sb("PST", [1, 64], F32); PSB = P.sb("PSB", [128, 64], F32); T64 = P.sb("T64b", [128, 64], F32)
            PSTK = P.sb("PSTK", [128, 1], F32); DF = P.sb("DF", [128, NT, 2], F32)
            PEC = P.sb("PEC", [64, 1], F32); BG = P.sb("BG", [64, NB], F32); CM = P.sb("CM", [64, NB], F32)
            BEF = P.sb("BEF", [128, NB], F32); IOP = P.sb("IOP", [128, 1], F32); ONE1 = P.sb("ONE1", [1, 1], F32)
            WQ = P.sb("WQ", [128, NB], F32)
            FILL = P.sb("FILL", [128, NB * 16], I32); TOKR = P.sb("TOKR", [128, NT, 16], I32)
            psd = P.ps("psd", [128, 512])
            P.dma('sp', BG[:], bgrid[:, :], writes=['BG']); P.dma('sp', IOP[:], iotap[:, :], writes=['IOP'])
            P.dma('sp', TOKR[:], tokrow[:, :, :], writes=['TOKR'])
            P.op('dve', lambda e: e.memset(ONE1[:], 1.0), writes=['ONE1'])
            P.op('pool', lambda e: e.memset(FILL[:], T), writes=['FILL'])
            P.dma('sp', SLOTTOK.rearrange("(p b) c -> p (b c)", p=128), FILL[:], reads=['FILL'], writes=['SLd'])
            P.op('dve', lambda e: e.tensor_scalar(out=CI[:], in0=CARRY[:], scalar1=127.0, scalar2=None, op0=ALU.add), reads=['CARRY'], writes=['CI'])
            P.op('dve', lambda e: e.tensor_scalar(out=CI[:], in0=CI[:], scalar1=7, scalar2=None, op0=ALU.arith_shift_right), reads=['CI'], writes=['CI'])
            P.op('dve', lambda e: e.tensor_scalar(out=CI[:], in0=CI[:], scalar1=7, scalar2=None, op0=ALU.logical_shift_left), reads=['CI'], writes=['CI'])
            P.op('dve', lambda e: e.tensor_copy(out=PAD[:], in_=CI[:]), reads=['CI'], writes=['PAD'])
            P.op('dve', lambda e: e.tensor_copy(out=PE_[0][:], in_=PAD[:]), reads=['PAD'], writes=['PE0'])
            cur = 0
            for st in range(6):
                sh = 1 << st
                nxt = 1 - cur
                P.op('dve', lambda e: e.tensor_copy(out=PE_[nxt][:, 0:sh], in_=PE_[cur][:, 0:sh]), reads=[f"PE{cur}"], writes=[f"PE{nxt}"])
                P.op('dve', lambda e: e.tensor_tensor(out=PE_[nxt][:, sh:64], in0=PE_[cur][:, sh:64], in1=PE_[cur][:, 0:64 - sh], op=ALU.add),
                     reads=[f"PE{cur}"], writes=[f"PE{nxt}"])
                cur = nxt
            PEND = PE_[cur]; pk = f"PE{cur}"
            P.op('dve', lambda e: e.tensor_tensor(out=PST[:], in0=PEND[:], in1=PAD[:], op=ALU.subtract), reads=[pk, 'PAD'], writes=['PST'])
            P.op('pe', lambda e: e.matmul(psd[:, 0:64], lhsT=ONES[0:1, :], rhs=PST[0:1, :], start=True, stop=True), reads=['CONST', 'PST'], writes=['psd'])
            P.op('act', lambda e: e.activation(out=PSB[:], in_=psd[:, 0:64], func=AF.Copy), reads=['psd'], writes=['PSB'])
            for tt in range(NT):
                for k in range(2):
                    P.op('dve', lambda e: e.tensor_tensor(out=T64[:], in0=MK[:, tt, k, :], in1=PSB[:], op=ALU.mult), reads=['MK', 'PSB'], writes=['T64b'])
                    P.op('dve', lambda e: e.reduce_sum(out=PSTK[:], in_=T64[:], axis=AX), reads=['T64b'], writes=['PSTK'])
                    P.op('dve', lambda e: e.tensor_tensor(out=DF[:, tt, k:k + 1], in0=PSTK[:], in1=RSEL[:, tt, k:k + 1], op=ALU.add), reads=['PSTK', 'RSEL'], writes=['DF'])
            P.op('dve', lambda e: e.tensor_copy(out=DESTI[:], in_=DF[:]), reads=['DF'], writes=['DESTI'])
            P.op('pe', lambda e: e.matmul(psd[0:64, 64:65], lhsT=PEND[0:1, :], rhs=ONE1[0:1, 0:1], start=True, stop=True), reads=[pk, 'ONE1', 'PSB'], writes=['psd'])
            P.op('act', lambda e: e.activation(out=PEC[:], in_=psd[0:64, 64:65], func=AF.Copy), reads=['psd'], writes=['PEC'])
            P.op('dve', lambda e: e.tensor_scalar(out=CM[:], in0=BG[:], scalar1=PEC[:, 0:1], scalar2=None, op0=ALU.is_ge), reads=['BG', 'PEC'], writes=['CM'])
            P.op('pe', lambda e: e.matmul(psd[:, 128:128 + NB], lhsT=ONES[0:64, :], rhs=CM[:], start=True, stop=True), reads=['CONST', 'CM', 'PEC'], writes=['psd'])
            P.op('dve', lambda e: e.tensor_scalar(out=BEF[:], in0=psd[:, 128:128 + NB], scalar1=63.0, scalar2=128.0, op0=ALU.min, op1=ALU.mult), reads=['psd'], writes=['BEF'])
            P.op('dve', lambda e: e.tensor_scalar(out=BEF[:], in0=BEF[:], scalar1=IOP[:, 0:1], scalar2=None, op0=ALU.add), reads=['BEF', 'IOP'], writes=['BEF'])
            for kq in range(4):
                P.op('dve', lambda e: e.tensor_scalar(out=WQ[:], in0=BEF[:], scalar1=4.0, scalar2=float(kq), op0=ALU.mult, op1=ALU.add), reads=['BEF'], writes=['WQ'])
                P.op('dve', lambda e: e.tensor_copy(out=WINI[:, :, kq], in_=WQ[:]), reads=['WQ'], writes=['WINI'])
            for h in range(2):
                P.op('dve', lambda e: e.tensor_scalar(out=WQ[:], in0=BEF[:], scalar1=2.0, scalar2=float(h), op0=ALU.mult, op1=ALU.add), reads=['BEF'], writes=['WQ'])
                P.op('dve', lambda e: e.tensor_copy(out=WOUTI[:, :, h], in_=WQ[:]), reads=['WQ'], writes=['WOUTI'])
            for tt in range(NT):
                for k in range(2):
                    P.idma(SLOTTOK[:, :], TOKR[:, tt, :], out_idx=DESTI[:, tt, k:k + 1], reads=['TOKR', 'DESTI', 'SLd'], writes=[f"SLs"])
        MKs.close()
        STB = P.sb("STB", [128, NB, 16], I32)
        with P.scope():
            XB = P.sb("XB", [128, D], F32); XBT = P.sb("XBT", [128, KC, 128], BF16)
            WW = [P.sb(f"WW{i}", [128, 8192], F32) for i in range(2)]
            WB = [P.sb(f"WBe{i}", [128, 8192], BF16) for i in range(3)]
            CENG = ['act', 'dve', 'act', 'dve', 'act', 'dve']
            SG = P.sb("SG", [128, 512], F32); AV = P.sb("AV", [128, 512], F32); AT = P.sb("AT", [128, 4, 128], BF16)
            YB = P.sb("YB", [128, D], BF16)
            psT = [P.ps(f"psT{i}", [128, 512]) for i in range(2)]
            psH = [P.ps(f"psH{i}", [128, 512]) for i in range(2)]
            psO = [P.ps(f"psO{i}", [128, 512]) for i in range(2)]
            P.dma('sp', STB[:], SLOTTOK.rearrange("(b p) c -> p b c", p=128), reads=['SLs', 'SLd'], writes=['STB'])
            wn = 0
            for b in range(NB):
                P.idma(XB[:], HN[:, :], in_idx=STB[:, b, 0:1], reads=['STB', 'HNd'], writes=['XB'])
                for k4 in range(8):
                    pt = psT[k4 % 2]
                    for q in range(4):
                        kc = k4 * 4 + q
                        P.op('pe', lambda e: e.transpose(out=pt[:, q * 128:(q + 1) * 128], in_=XB[:, kc * 128:(kc + 1) * 128], identity=IDENT),
                             reads=['XB', 'CONST'], writes=[f"psT{k4 % 2}"])
                    dstv = XBT[:, k4 * 4:(k4 + 1) * 4, :].rearrange("p a t -> p (a t)")
                    if k4 % 2 == 0:
                        P.op('act', lambda e: e.activation(out=dstv, in_=pt[:, :], func=AF.Copy), reads=[f"psT{k4 % 2}"], writes=['XBT'])
                    else:
                        P.op('dve', lambda e: e.tensor_copy(out=dstv, in_=pt[:, :]), reads=[f"psT{k4 % 2}"], writes=['XBT'])
                for kq in range(4):
                    eng = CENG[wn % 6]; wi = wn % 2; bi = wn % 3; wn += 1
                    P.idma(WW[wi][:], ewin[:, :], in_idx=WINI[:, b, kq:kq + 1], reads=['WINI'], writes=[f"WW{wi}"])
                    if eng == 'act':
                        P.op('act', lambda e: e.activation(out=WB[bi][:], in_=WW[wi][:], func=AF.Copy), reads=[f"WW{wi}"], writes=[f"WBe{bi}"])
                    else:
                        P.op(eng, lambda e: e.tensor_copy(out=WB[bi][:], in_=WW[wi][:]), reads=[f"WW{wi}"], writes=[f"WBe{bi}"])
                    for kcl in range(8):
                        kc = kq * 8 + kcl
                        for hf in range(2):
                            P.op('pe', lambda e: e.matmul(psH[hf][:, :], lhsT=XBT[:, kc, :], rhs=WB[bi][:, kcl * 1024 + hf * 512:kcl * 1024 + (hf + 1) * 512],
                                                          start=(kc == 0), stop=(kc == KC - 1)),
                                 reads=['XBT', f"WBe{bi}"], writes=[f"psH{hf}"])
                P.op('act', lambda e: e.activation(out=SG[:], in_=psH[0][:, :], func=AF.Silu), reads=["psH0"], writes=['SG'])
                P.op('dve', lambda e: e.tensor_tensor(out=AV[:], in0=SG[:], in1=psH[1][:, :], op=ALU.mult), reads=['SG', "psH1"], writes=['AV'])
                for q in range(4):
                    P.op('pe', lambda e: e.transpose(out=psT[0][:, q * 128:(q + 1) * 128], in_=AV[:, q * 128:(q + 1) * 128], identity=IDENT),
                         reads=['AV', 'CONST'], writes=["psT0"])
                P.op('act', lambda e: e.activation(out=AT[:].rearrange("p a t -> p (a t)"), in_=psT[0][:, :], func=AF.Copy), reads=["psT0"], writes=['AT'])
                for h in range(2):
                    eng = CENG[wn % 6]; wi = wn % 2; bi = wn % 3; wn += 1
                    P.idma(WW[wi][:], ewout[:, :], in_idx=WOUTI[:, b, h:h + 1], reads=['WOUTI'], writes=[f"WW{wi}"])
                    if eng == 'act':
                        P.op('act', lambda e: e.activation(out=WB[bi][:], in_=WW[wi][:], func=AF.Copy), reads=[f"WW{wi}"], writes=[f"WBe{bi}"])
                    else:
                        P.op(eng, lambda e: e.tensor_copy(out=WB[bi][:], in_=WW[wi][:]), reads=[f"WW{wi}"], writes=[f"WBe{bi}"])
                    for nb_ in range(4):
                        po = (h * 4 + nb_) % 2
                        for kq in range(4):
                            P.op('pe', lambda e: e.matmul(psO[po][:, :], lhsT=AT[:, kq, :], rhs=WB[bi][:, kq * 2048 + nb_ * 512:kq * 2048 + (nb_ + 1) * 512],
                                                          start=(kq == 0), stop=(kq == 3)),
                                 reads=['AT', f"WBe{bi}"], writes=[f"psO{po}"])
                        dsl = slice(h * 2048 + nb_ * 512, h * 2048 + (nb_ + 1) * 512)
                        if nb_ % 2 == 0:
                            P.op('act', lambda e: e.activation(out=YB[:, dsl], in_=psO[po][:, :], func=AF.Copy), reads=[f"psO{po}"], writes=['YB'])
                        else:
                            P.op('dve', lambda e: e.tensor_copy(out=YB[:, dsl], in_=psO[po][:, :]), reads=[f"psO{po}"], writes=['YB'])
                P.dma('sp', YS[b * 128:(b + 1) * 128, :], YB[:], reads=['YB'], writes=['YSd'])
        with P.scope():
            XR = [P.sb(f"XR{i}", [128, D], F32) for i in range(2)]
            Y0 = [P.sb(f"Y0{i}", [128, D], BF16) for i in range(2)]
            Y1 = [P.sb(f"Y1{i}", [128, D], BF16) for i in range(2)]
            JUNK = P.sb("JUNK", [128, D], BF16)
            SS = P.sb("SS", [128, 1], F32); RS = P.sb("RS", [128, 1], F32); EPSC = P.sb("EPSC", [128, 1], F32)
            ROWT = P.sb("ROWT", [1, D], F32)
            psl = P.ps("psl", [128, 512])
            P.op('dve', lambda e: e.memset(EPSC[:], EPS), writes=['EPSC'])
            if final:
                bcast_row(GBC, 'GBC', grows[1:2, :], D, psl, ROWT)
            for tt in range(NT):
                i = tt % 2
                tsl = slice(tt * 128, (tt + 1) * 128)
                P.dma('sp', XR[i][:], X1[tsl, :], writes=[f"XR{i}"])
                P.idma(Y0[i][:], YS[:, :], in_idx=DESTI[:, tt, 0:1], reads=['DESTI'], writes=[f"Y0{i}"])
                P.idma(Y1[i][:], YS[:, :], in_idx=DESTI[:, tt, 1:2], reads=['DESTI'], writes=[f"Y1{i}"])
                P.op('dve', lambda e: e.scalar_tensor_tensor(out=XR[i][:], in0=Y0[i][:], scalar=GATES[:, tt, 0:1], in1=XR[i][:], op0=ALU.mult, op1=ALU.add),
                     reads=[f"Y0{i}", 'GATES', f"XR{i}"], writes=[f"XR{i}"])
                P.op('dve', lambda e: e.scalar_tensor_tensor(out=XR[i][:], in0=Y1[i][:], scalar=GATES[:, tt, 1:2], in1=XR[i][:], op0=ALU.mult, op1=ALU.add),
                     reads=[f"Y1{i}", 'GATES', f"XR{i}"], writes=[f"XR{i}"])
                if final:
                    P.op('act', lambda e: e.activation(out=JUNK[:], in_=XR[i][:], func=AF.Square, accum_out=SS[:, 0:1]), reads=[f"XR{i}"], writes=['JUNK', 'SS'])
                    P.op('act', lambda e: e.activation(out=RS[:], in_=SS[:], func=AF.Ln, bias=EPSC[:, 0:1], scale=1.0 / D), reads=['SS', 'EPSC'], writes=['RS'])
                    P.op('act', lambda e: e.activation(out=RS[:], in_=RS[:], func=AF.Exp, scale=-0.5), reads=['RS'], writes=['RS'])
                    P.op('dve', lambda e: e.scalar_tensor_tensor(out=XR[i][:], in0=XR[i][:], scalar=RS[:, 0:1], in1=GBC[:], op0=ALU.mult, op1=ALU.mult),
                         reads=[f"XR{i}", 'RS', 'GBC'], writes=[f"XR{i}"])
                P.dma('act', out[tsl, :], XR[i][:], reads=[f"XR{i}"], is_output=True)

    if not BACK:
        P.barrier()
        MKs.close()
    P.finish('sp')
    print("L3 ninstr", P.ninstr, "nsem", P.nsem)
    es.close()
    return nc

def l3_inputs(L, xT, mixT, w, T):
    f32 = np.float32
    NB = n_moe_blocks(T); NT = T // 128
    cat = np.concatenate([w['router_group_w'][L], w['router_expert_w'][L]], 1)
    m = {"xT": xT, "mixT": mixT,
         "cvcols": np.stack([w['conv_ln_g'][L].reshape(8, 128).T, w['conv_ln_b'][L].reshape(8, 128).T], 1),
         "pwt": w['conv_pw'][L].reshape(8, 128, 8, 128).transpose(2, 1, 0, 3),
         "wot": w['w_out'][L].reshape(32, 128, 32, 128).transpose(2, 1, 0, 3),
         "fgain": w['ffn_norm'][L].reshape(32, 128).T,
         "grows": np.stack([w['ffn_norm'][L], w['final_norm']], 0),
         "wr": cat.reshape(32, 128, 72).transpose(1, 0, 2),
         "rbias": np.concatenate([w['router_group_b'][L], w['router_expert_b'][L]])[None, :],
         "ewin": w['expert_w_in'][L].reshape(64, 4, 8, 128, 1024).transpose(0, 3, 1, 2, 4).reshape(64 * 128 * 4, 8192),
         "ewout": w['expert_w_out'][L].reshape(64, 4, 128, 2, 2048).transpose(0, 2, 3, 1, 4).reshape(64 * 128 * 2, 8192),
         "consts": np.stack([np.ones((128, 128), f32), np.eye(128, dtype=f32), np.triu(np.ones((128, 128), f32), 1)], 0),
         "bgrid": np.broadcast_to(128.0 * np.arange(NB, dtype=f32)[None, :], (64, NB)),
         "iotap": np.arange(128, dtype=f32)[:, None]}
    m = {k: np.ascontiguousarray(v, dtype=f32) for k, v in m.items()}
    m["tokrow"] = np.ascontiguousarray(np.broadcast_to((np.arange(NT, dtype=np.int32)[None, :, None] * 128 + np.arange(128, dtype=np.int32)[:, None, None]), (128, NT, 16)))
    return m


def kernel(**inputs):
    w = {k: np.asarray(v) for k, v in inputs.items()}
    x = np.asarray(w['x'][0], dtype=np.float32)
    T = x.shape[0]
    TL = T // 8
    xT = np.ascontiguousarray(x.T)
    vT = None
    x2 = None
    FK = ["consts", "grows", "cvcols", "pwt", "wot", "fgain", "wr", "rbias"]
    BK = ["consts", "grows", "ewin", "ewout", "bgrid", "iotap", "tokrow"]
    for L in range(2):
        nc1 = build_l1(T, L > 0)
        maps = []
        for c in range(8):
            m = l1_core_inputs(c, xT, L, w)
            if L > 0:
                m["vfT"] = vT[c]
            maps.append(m)
        r = run_bass_kernel_spmd(nc1, maps, core_ids=list(range(8))).results
        del maps
        mixT = np.empty((4096, T), np.float32)
        for c in range(8):
            g, half = c // 2, c % 2
            mixT[g * 256 + half * 128:g * 256 + (half + 1) * 128] = r[c]["ypoolT"]
            mixT[1024 + c * 128:1024 + (c + 1) * 128] = r[c]["hcT"]
            mixT[2048 + c * 256:2048 + (c + 1) * 256] = r[c]["yrwT"]
        if L == 0:
            vT = [np.ascontiguousarray(r[c]["vT"], dtype=np.float32) for c in range(8)]
        del r
        m3 = l3_inputs(L, xT, mixT, w, T)
        nc2 = build_l3(TL, False, 'front')
        maps = []
        for c in range(8):
            mm = {kk: m3[kk] for kk in FK}
            mm["xT"] = np.ascontiguousarray(xT[:, c * TL:(c + 1) * TL])
            mm["mixT"] = np.ascontiguousarray(mixT[:, c * TL:(c + 1) * TL])
            maps.append(mm)
        r = run_bass_kernel_spmd(nc2, maps, core_ids=list(range(8))).results
        del maps
        mb = {kk: m3[kk] for kk in BK}
        del m3
        mb["x1_in"] = np.ascontiguousarray(np.concatenate([r[c]["x1_out"] for c in range(8)], 0), dtype=np.float32)
        mb["hn_in"] = np.ascontiguousarray(np.concatenate([r[c]["hn_out"] for c in range(8)] + [np.zeros((128, 4096), np.float32)], 0), dtype=np.float32)
        mb["mk_in"] = np.ascontiguousarray(np.concatenate([r[c]["mk_out"] for c in range(8)], 1), dtype=np.float32)
        mb["gates_in"] = np.ascontiguousarray(np.concatenate([r[c]["gates_out"] for c in range(8)], 1), dtype=np.float32)
        del r
        nc3 = build_l3(T, L == 1, 'back')
        x2 = np.asarray(run_bass_kernel_spmd(nc3, [mb], core_ids=[0]).results[0]["out"], dtype=np.float32)
        del mb
        xT = np.ascontiguousarray(x2.T)
    return x2[None]
```

```python
import numpy as np
from contextlib import ExitStack
import concourse.bass as bass
import concourse.mybir as mybir
from concourse.bass_utils import run_bass_kernel_spmd

F32 = mybir.dt.float32
BF16 = mybir.dt.bfloat16
I32 = mybir.dt.int32
AF = mybir.ActivationFunctionType
ALU = mybir.AluOpType


class Prog:
    EPOCH = 12000
    NDS = 40

    def __init__(self, nc, es):
        self.nc, self.es = nc, es
        self.es_root = es
        self.E = {'pe': nc.tensor, 'act': nc.scalar, 'dve': nc.vector,
                  'pool': nc.gpsimd, 'sp': nc.sync}
        self.cur = {}
        self.seen = {e: {} for e in self.E}
        self.bufs = {}
        self.nsem = 0
        self.dsems = []
        self.dcnt = []
        self.di = 0
        self.out_toks = []
        self.ninstr = 0

    def _newsem(self):
        s = self.es_root.enter_context(self.nc.semaphore(f"s{self.nsem}"))
        self.nsem += 1
        return s

    def sb(self, name, shape, dt):
        self.nten = getattr(self, 'nten', 0) + 1
        return self.es.enter_context(self.nc.sbuf_tensor(f"{name}_u{self.nten}", shape, dt))

    def ps(self, name, shape, dt=F32):
        self.nten = getattr(self, 'nten', 0) + 1
        return self.es.enter_context(self.nc.psum_tensor(f"{name}_u{self.nten}", shape, dt))

    def _tok(self, e):
        if e not in self.cur or self.cur[e][1] >= self.EPOCH:
            self.cur[e] = [self._newsem(), 0]
        self.cur[e][1] += 1
        return (self.cur[e][0], self.cur[e][1])

    def _wait(self, e, tok):
        sem, val = tok
        k = id(sem)
        if self.seen[e].get(k, 0) >= val:
            return
        self.E[e].wait_ge(sem, val)
        self.ninstr += 1
        self.seen[e][k] = val

    def _deps(self, e, reads, writes):
        toks = []
        for k in reads:
            b = self.bufs.get(k)
            if b and b['w']:
                toks.append(b['w'])
        for k in writes:
            b = self.bufs.get(k)
            if b:
                if b['w']:
                    toks.append(b['w'])
                toks += list(b['r'].values())
        own = self.cur.get(e)
        for t in toks:
            if e == 'pe' and own is not None and t[0] is own[0]:
                continue
            self._wait(e, t)

    def _commit(self, tok, reads, writes):
        for k in reads:
            b = self.bufs.setdefault(k, {'w': None, 'r': {}})
            o = b['r'].get(id(tok[0]))
            if o is None or o[1] < tok[1]:
                b['r'][id(tok[0])] = tok
        for k in writes:
            self.bufs[k] = {'w': tok, 'r': {}}

    def op(self, e, fn, reads=(), writes=()):
        self._deps(e, reads, writes)
        tok = self._tok(e)
        fn(self.E[e]).then_inc(tok[0], 1)
        self.ninstr += 1
        self._commit(tok, reads, writes)
        return tok

    def dma(self, q, out, in_, reads=(), writes=(), is_output=False, **kw):
        self._deps(q, reads, writes)
        if len(self.dsems) < self.NDS:
            self.dsems.append(self._newsem())
            self.dcnt.append(0)
            i = len(self.dsems) - 1
        else:
            i = self.di
            self.di = (self.di + 1) % self.NDS
            self._wait(q, (self.dsems[i], self.dcnt[i]))
        self.dcnt[i] += 16
        tok = (self.dsems[i], self.dcnt[i])
        self.E[q].dma_start(out=out, in_=in_, **kw).then_inc(tok[0], 16)
        self.ninstr += 1
        self._commit(tok, reads, writes)
        if is_output:
            self.out_toks.append(tok)
        return tok

    def finish(self, q='sp'):
        for t in self.out_toks:
            self._wait(q, t)

    def barrier(self):
        toks = [(v[0], v[1]) for v in self.cur.values()] + [(s_, c) for s_, c in zip(self.dsems, self.dcnt) if c > 0]
        for e in self.E:
            for t in toks:
                self._wait(e, t)
        self.bufs = {}

    def scope(self):
        return _Scope(self)


class _Scope:
    def __init__(self, P):
        self.P = P

    def __enter__(self):
        from contextlib import ExitStack
        self.old = self.P.es
        self.P.es = ExitStack()
        return self

    def __exit__(self, *a):
        self.P.barrier()
        self.P.es.close()
        self.P.es = self.old
        return False


def _idma(self, out, in_, out_idx=None, in_idx=None, reads=(), writes=(), is_output=False):
    q = 'pool'
    self._deps(q, reads, writes)
    if len(self.dsems) < self.NDS:
        self.dsems.append(self._newsem()); self.dcnt.append(0)
        i = len(self.dsems) - 1
    else:
        i = self.di
        self.di = (self.di + 1) % self.NDS
        self._wait(q, (self.dsems[i], self.dcnt[i]))
    self.dcnt[i] += 16
    tok = (self.dsems[i], self.dcnt[i])
    self.nc.gpsimd.indirect_dma_start(
        out=out, out_offset=(bass.IndirectOffsetOnAxis(ap=out_idx, axis=0) if out_idx is not None else None),
        in_=in_, in_offset=(bass.IndirectOffsetOnAxis(ap=in_idx, axis=0) if in_idx is not None else None)).then_inc(tok[0], 16)
    self.ninstr += 1
    self._commit(tok, reads, writes)
    if is_output:
        self.out_toks.append(tok)
    return tok


Prog.idma = _idma


D = 4096
KC = 32
HALO = 32
TB = 1024
NCT = 15
KW = 31
HPC = 4
CH = 32
G = CH // 8
EPS = 1e-6
GN_EPS = 64e-5
T_POOL, T_CV, T_CG, T_XW, T_XA, T_XG0, T_XG1, T_VD, T_R0 = 0, 2, 3, 4, 5, 6, 7, 8, 9


def build_l1(T, layer1):
    nc = bass.Bass("TRN2", target_bir_lowering=False)
    NBLK = T // TB
    TE = TB + HALO
    din = lambda n, s, dt=F32: nc.dram_tensor(n, s, dt, kind="ExternalInput").ap()
    dout = lambda n, s, dt=F32: nc.dram_tensor(n, s, dt, kind="ExternalOutput").ap()
    dscr = lambda n, s, dt=F32: nc.dram_tensor(n, s, dt).ap()
    xT = din("xT", [D, T]); gain = din("gain", [128, KC]); wt = din("wt", [NCT, 128, KC, 128])
    consts = din("consts", [4, 128, 128])
    pwt = din("pwt", [128, 2, 128]); pcol = din("pcol", [128, 5]); pinv = din("pinv", [128, 4, 16])
    dwT = din("dwT", [128, KW + 1])
    mixc = din("mixc", [128, 11]); cols = din("cols", [128, 8, 2]); ups = din("ups", [2, 128, 5, 128])
    lncols = din("lncols", [128, 2, 2])
    vfT = din("vfT", [256, T]) if layer1 else None
    ypoolT = dout("ypoolT", [128, T]); hcT = dout("hcT", [128, T]); yrwT = dout("yrwT", [256, T]); vT = dout("vT", [256, T])
    XG = dscr("XG", [128, KC, T], BF16)
    PS = dscr("PS", [NCT * 128, HALO + T])
    ATM = dscr("ATM", [T, HPC, 64], BF16); BTM = dscr("BTM", [T, HPC, 64], BF16)
    KRV = dscr("KRV", [HPC, T, 129], BF16)
    RPT = dscr("RPT", [64, HPC, T]); WTS = dscr("WTS", [64, HPC, T])
    BONT = dscr("BONT", [256, T]); GT = dscr("GT", [256, T])
    YTM = dscr("YTM", [HPC, T, 64], BF16)
    es = ExitStack(); P = Prog(nc, es)
    RSTD = P.sb("RSTD", [128, T], F32)
    CONST = P.sb("CONST", [128, 4, 128], F32)
    IDB = P.sb("IDB", [128, 128], BF16)
    P.dma('sp', CONST[:], consts.rearrange("c p n -> p c n"), writes=['CONST'])
    P.op('dve', lambda e: e.tensor_copy(out=IDB[:], in_=CONST[:, 3, :]), reads=['CONST'], writes=['IDB'])
    ONES, BONES, BONES64, IDENT = CONST[:, 0, :], CONST[:, 1, :], CONST[:, 2, :], CONST[:, 3, :]

    with P.scope():
        XGs = P.sb("XGs", [128, KC, TB], BF16)
        XC = [P.sb(f"XC{i}", [128, TB], F32) for i in range(2)]
        SQ = [P.sb(f"SQ{i}", [128, TB], F32) for i in range(2)]
        GN = P.sb("GN", [128, KC], F32); EPSC = P.sb("EPSC", [128, 1], F32)
        psA = [P.ps(f"psA{b}", [128, 512]) for b in range(2)]
        P.dma('sp', GN[:], gain[:, :], writes=['GN'])
        P.op('dve', lambda e: e.memset(EPSC[:], EPS), writes=['EPSC'])
        for tb in range(NBLK):
            tsl = slice(tb * TB, (tb + 1) * TB)
            for kc in range(KC):
                i = kc % 2
                P.dma('sp' if i == 0 else 'act', XC[i][:], xT[kc * 128:(kc + 1) * 128, tsl], writes=[f"XC{i}"])
                P.op('act', lambda e: e.activation(out=SQ[i][:], in_=XC[i][:], func=AF.Square), reads=[f"XC{i}"], writes=[f"SQ{i}"])
                for b in range(2):
                    P.op('pe', lambda e: e.matmul(psA[b][:, :], lhsT=ONES, rhs=SQ[i][:, b * 512:(b + 1) * 512],
                                                  start=(kc == 0), stop=(kc == KC - 1)), reads=['CONST', f"SQ{i}"], writes=[f"psA{b}"])
                P.op('dve' if i == 0 else 'pool',
                     lambda e: e.tensor_scalar(out=XGs[:, kc, :], in0=XC[i][:], scalar1=GN[:, kc:kc + 1], scalar2=None, op0=ALU.mult),
                     reads=[f"XC{i}", 'GN'], writes=[f"XGs{kc}"])
            for b in range(2):
                sl = slice(tb * TB + b * 512, tb * TB + (b + 1) * 512)
                P.op('act', lambda e: e.activation(out=RSTD[:, sl], in_=psA[b][:, :], func=AF.Ln, bias=EPSC[:, 0:1], scale=1.0 / D),
                     reads=[f"psA{b}", 'EPSC'], writes=['RSTD'])
            P.op('act', lambda e: e.activation(out=RSTD[:, tsl], in_=RSTD[:, tsl], func=AF.Exp, scale=-0.5), reads=['RSTD'], writes=['RSTD'])
            P.dma('pool', XG[:, :, tsl], XGs[:], reads=[f"XGs{k_}" for k_ in range(KC)], writes=["XGd"])
    with P.scope():
        WF = [P.sb(f"WF{i}", [128, KC, 128], F32) for i in range(2)]
        WB = [P.sb(f"WB{i}", [128, KC, 128], BF16) for i in range(4)]
        XB = [P.sb(f"XB{i}", [128, KC, 512], BF16) for i in range(2)]
        OT = [P.sb(f"OT{i}", [128, 512], F32) for i in range(4)]
        ZT = P.sb("ZT", [128, HALO], F32)
        psB = [P.ps(f"psB{i}", [128, 512]) for i in range(4)]
        P.op('dve', lambda e: e.memset(ZT[:], 0.0), writes=['ZT'])
        for ct in range(NCT):
            P.dma('sp', PS[ct * 128:(ct + 1) * 128, 0:HALO], ZT[:], reads=['ZT'], writes=[f"PSd{ct}"])
        n = 0
        for g0 in range(0, NCT, 4):
            cts = list(range(g0, min(g0 + 4, NCT)))
            for qi, ct in enumerate(cts):
                i = qi % 2
                P.dma('act', WF[i][:], wt[ct], writes=[f"WF{i}"])
                P.op('pool', lambda e: e.tensor_copy(out=WB[qi][:], in_=WF[i][:]), reads=[f"WF{i}"], writes=[f"WB{qi}"])
            for blk in range(T // 512):
                xi = blk % 2
                bsl = slice(blk * 512, (blk + 1) * 512)
                P.dma('sp', XB[xi][:], XG[:, :, bsl], reads=["XGd"], writes=[f"XB{xi}"])
                for qi, ct in enumerate(cts):
                    pb = n % 4; n += 1
                    for kc in range(KC):
                        P.op('pe', lambda e: e.matmul(psB[pb][:, :], lhsT=WB[qi][:, kc, :], rhs=XB[xi][:, kc, :],
                                                      start=(kc == 0), stop=(kc == KC - 1)),
                             reads=[f"WB{qi}", f"XB{xi}"] if kc == 0 else [], writes=[f"psB{pb}"])
                    P.op('dve', lambda e: e.tensor_tensor(out=OT[pb][:], in0=psB[pb][:, :], in1=RSTD[:, bsl], op=ALU.mult),
                         reads=[f"psB{pb}", 'RSTD'], writes=[f"OT{pb}"])
                    P.dma('pool', PS[ct * 128:(ct + 1) * 128, HALO + blk * 512:HALO + (blk + 1) * 512], OT[pb][:],
                          reads=[f"OT{pb}"], writes=[f"PSd{ct}"])
                P._commit((P.cur['pe'][0], P.cur['pe'][1]), [f"XB{xi}"] + [f"WB{q}" for q in range(len(cts))], [])
    psd = [f"PSd{ct}" for ct in range(NCT)]
    with P.scope():
        U = [P.sb(f"U{i}", [128, TE], F32) for i in range(2)]
        SW = [P.sb(f"SW{i}", [128, TE], F32) for i in range(4)]
        TMP = P.sb("TMP", [128, TB], F32); T16 = P.sb("T16", [128, 16], F32)
        MX = [P.sb(f"MX{i}", [128, TB], BF16) for i in range(2)]
        PWF = P.sb("PWF", [128, 2, 128], F32); PWB = P.sb("PWB", [128, 2, 128], BF16)
        PCOL = P.sb("PCOL", [128, 5], F32); PINV = P.sb("PINV", [128, 4, 16], F32)
        OTp = P.sb("OTp", [128, TB], F32)
        psP = [P.ps(f"psP{i}", [128, 512]) for i in range(2)]
        P.dma('sp', PWF[:], pwt[:, :, :], writes=['PWF']); P.dma('sp', PCOL[:], pcol[:, :], writes=['PCOL'])
        P.dma('sp', PINV[:], pinv[:, :, :], writes=['PINV'])
        P.op('pool', lambda e: e.tensor_copy(out=PWB[:], in_=PWF[:]), reads=['PWF'], writes=['PWB'])
        for tb in range(NBLK):
            for c2 in range(2):
                P.dma('sp', U[c2][:], PS[(T_POOL + c2) * 128:(T_POOL + c2 + 1) * 128, tb * TB:tb * TB + TE], reads=psd, writes=[f"U{c2}"])
                src, skey = U[c2], f"U{c2}"
                for st in range(4):
                    sh = 1 << st
                    P.op('dve' if st % 2 == 0 else 'pool',
                         lambda e: e.tensor_tensor(out=SW[st][:, sh:TE], in0=src[:, sh:TE], in1=src[:, 0:TE - sh], op=ALU.add),
                         reads=[skey], writes=[f"SW{st}"])
                    src, skey = SW[st], f"SW{st}"
                P.op('dve', lambda e: e.tensor_scalar(out=TMP[:], in0=SW[0][:, HALO:TE], scalar1=PCOL[:, 0:1], scalar2=None, op0=ALU.mult),
                     reads=["SW0", 'PCOL'], writes=['TMP'])
                for st in range(1, 4):
                    P.op('dve', lambda e: e.scalar_tensor_tensor(out=TMP[:], in0=SW[st][:, HALO:TE], scalar=PCOL[:, st:st + 1], in1=TMP[:],
                                                                 op0=ALU.mult, op1=ALU.add), reads=[f"SW{st}", 'PCOL', 'TMP'], writes=['TMP'])
                if tb == 0:
                    P.op('dve', lambda e: e.tensor_tensor(out=TMP[:, 0:16], in0=SW[0][:, HALO:HALO + 16], in1=PINV[:, 0, :], op=ALU.mult),
                         reads=["SW0", 'PINV', 'TMP'], writes=['TMP'])
                    for st in range(1, 4):
                        P.op('dve', lambda e: e.tensor_tensor(out=T16[:], in0=SW[st][:, HALO:HALO + 16], in1=PINV[:, st, :], op=ALU.mult),
                             reads=[f"SW{st}", 'PINV'], writes=['T16'])
                        P.op('dve', lambda e: e.tensor_tensor(out=TMP[:, 0:16], in0=TMP[:, 0:16], in1=T16[:], op=ALU.add),
                             reads=['TMP', 'T16'], writes=['TMP'])
                P.op('dve', lambda e: e.tensor_tensor(out=MX[c2][:], in0=TMP[:], in1=U[c2][:, HALO:TE], op=ALU.subtract),
                     reads=['TMP', f"U{c2}"], writes=[f"MX{c2}"])
            for b in range(2):
                for c2 in range(2):
                    P.op('pe', lambda e: e.matmul(psP[b][:, :], lhsT=PWB[:, c2, :], rhs=MX[c2][:, b * 512:(b + 1) * 512],
                                                  start=(c2 == 0), stop=(c2 == 1)), reads=['PWB', f"MX{c2}"], writes=[f"psP{b}"])
                P.op('act', lambda e: e.activation(out=OTp[:, b * 512:(b + 1) * 512], in_=psP[b][:, :], func=AF.Copy, scale=PCOL[:, 4:5]),
                     reads=[f"psP{b}", 'PCOL'], writes=['OTp'])
            P.dma('pool', ypoolT[:, tb * TB:(tb + 1) * TB], OTp[:], reads=['OTp'], is_output=True)
    with P.scope():
        V = [P.sb(f"V{i}", [128, TE], F32) for i in range(2)]
        Gt = [P.sb(f"G{i}", [128, TE], F32) for i in range(2)]
        H = [P.sb(f"H{i}", [128, TE], F32) for i in range(2)]
        HC = [P.sb(f"HC{i}", [128, TB], F32) for i in range(2)]
        DW = P.sb("DW", [128, KW + 1], F32)
        P.dma('sp', DW[:], dwT[:, :], writes=['DW'])
        o0 = HALO - (KW - 1)
        for tb in range(NBLK):
            i = tb % 2
            P.dma('sp', V[i][:], PS[T_CV * 128:(T_CV + 1) * 128, tb * TB:tb * TB + TE], reads=psd, writes=[f"V{i}"])
            P.dma('act', Gt[i][:], PS[T_CG * 128:(T_CG + 1) * 128, tb * TB:tb * TB + TE], reads=psd, writes=[f"G{i}"])
            P.op('act', lambda e: e.activation(out=Gt[i][:], in_=Gt[i][:], func=AF.Sigmoid), reads=[f"G{i}"], writes=[f"G{i}"])
            P.op('pool', lambda e: e.tensor_tensor(out=H[i][:], in0=V[i][:], in1=Gt[i][:], op=ALU.mult), reads=[f"V{i}", f"G{i}"], writes=[f"H{i}"])
            P.op('dve', lambda e: e.tensor_scalar(out=HC[i][:], in0=H[i][:, o0:o0 + TB], scalar1=DW[:, 0:1], scalar2=DW[:, KW:KW + 1],
                                                  op0=ALU.mult, op1=ALU.add), reads=[f"H{i}", 'DW'], writes=[f"HC{i}"])
            for jj in range(1, KW):
                P.op('dve', lambda e: e.scalar_tensor_tensor(out=HC[i][:], in0=H[i][:, o0 + jj:o0 + jj + TB], scalar=DW[:, jj:jj + 1],
                                                             in1=HC[i][:], op0=ALU.mult, op1=ALU.add),
                     reads=[f"H{i}", 'DW', f"HC{i}"], writes=[f"HC{i}"])
            P.dma('pool', hcT[:, tb * TB:(tb + 1) * TB], HC[i][:], reads=[f"HC{i}"], is_output=True)
    with P.scope():
        PT = [P.sb(f"PT{i}", [128, TE], F32) for i in range(3)]
        NL = 5
        LO = [P.sb(f"LO{i}", [128, TB], BF16) for i in range(NL)]
        names = ["R", "K", "V", "D", "E1", "Wd", "A", "G", "KK", "T1", "T2", "AA", "BBv", "Kp", "Rp", "RK", "BON"]
        Ft = {nn: P.sb("F_" + nn, [128, TB], F32) for nn in names}
        UPF = P.sb("UPF", [128, 5, 128], F32); UPB = [P.sb(f"UPB{j}", [128, 5, 128], BF16) for j in range(2)]
        MIX = P.sb("MIX", [128, 11], F32); COL = P.sb("COL", [128, 8, 2], F32)
        TMA = P.sb("TMA", [128, 8, 2, 64], BF16); TMB = P.sb("TMB", [128, 8, 2, 64], BF16); TMK = P.sb("TMK", [128, 8, 2, 129], BF16)
        ps = [P.ps(f"ps{i}", [128, 512]) for i in range(8)]
        P.dma('sp', MIX[:], mixc[:, :], writes=['MIX']); P.dma('sp', COL[:], cols[:, :, :], writes=['COL'])
        P.op('dve', lambda e: e.tensor_scalar(out=COL[:, 0, :], in0=COL[:, 0, :], scalar1=-1.0, scalar2=None, op0=ALU.mult), reads=['COL'], writes=['COL'])
        P.op('dve', lambda e: e.tensor_scalar(out=COL[:, 4, :], in0=COL[:, 3, :], scalar1=-1.0, scalar2=1.0, op0=ALU.mult, op1=ALU.add), reads=['COL'], writes=['COL'])
        for j in range(2):
            P.dma('act', UPF[:], ups[j], writes=['UPF'])
            P.op('pool', lambda e: e.tensor_copy(out=UPB[j][:], in_=UPF[:]), reads=['UPF'], writes=[f"UPB{j}"])
        pc = [0]

        def nps():
            pc[0] = (pc[0] + 1) % 8
            return pc[0]

        def shift(dst, dkey, i, mcol):
            P.op('pool', lambda e: e.tensor_tensor(out=Ft["D"][:], in0=PT[i][:, HALO - 1:TE - 1], in1=PT[i][:, HALO:TE], op=ALU.subtract),
                 reads=[f"PT{i}"], writes=["D"])
            P.op('dve', lambda e: e.scalar_tensor_tensor(out=dst[:], in0=Ft["D"][:], scalar=MIX[:, mcol:mcol + 1], in1=PT[i][:, HALO:TE],
                                                         op0=ALU.mult, op1=ALU.add), reads=["D", f"PT{i}", 'MIX'], writes=[dkey])

        def bsum(src, skey):
            res = []
            for b in range(2):
                pb = nps()
                P.op('pe', lambda e: e.matmul(ps[pb][:, :], lhsT=BONES, rhs=src[:, b * 512:(b + 1) * 512], start=True, stop=True),
                     reads=['CONST', skey], writes=[f"ps{pb}"])
                res.append(pb)
            return res

        for tb in range(NBLK):
            tsl = slice(tb * TB, (tb + 1) * TB)
            for l, trow in enumerate([T_XW, T_XA, T_XG0, T_XG1, T_VD]):
                if l == 4 and not layer1:
                    continue
                P.dma('sp', PT[0][:], PS[trow * 128:(trow + 1) * 128, tb * TB:tb * TB + TE], reads=psd, writes=["PT0"])
                shift(Ft["T1"], "T1", 0, 6 + l)
                fn = AF.Tanh if l == 0 else (AF.Copy if l in (1, 4) else AF.Sigmoid)
                P.op('act', lambda e: e.activation(out=LO[l][:], in_=Ft["T1"][:], func=fn), reads=["T1"], writes=[f"LO{l}"])
            for j in range(2):
                for i, nn in enumerate(["R", "K", "V"]):
                    trow = T_R0 + 3 * j + i
                    P.dma('sp', PT[i][:], PS[trow * 128:(trow + 1) * 128, tb * TB:tb * TB + TE], reads=psd, writes=[f"PT{i}"])
                    shift(Ft[nn], nn, i, 3 * j + i)
                c = lambda q: COL[:, q, j:j + 1]
                for b in range(2):
                    sl = slice(b * 512, (b + 1) * 512)
                    pw, pa, pg = nps(), nps(), nps()
                    P.op('pe', lambda e: e.matmul(ps[pw][:, :], lhsT=UPB[j][:, 0, :], rhs=LO[0][:, sl], start=True, stop=True), reads=[f"UPB{j}", 'LO0'], writes=[f"ps{pw}"])
                    P.op('pe', lambda e: e.matmul(ps[pa][:, :], lhsT=UPB[j][:, 1, :], rhs=LO[1][:, sl], start=True, stop=True), reads=[f"UPB{j}", 'LO1'], writes=[f"ps{pa}"])
                    P.op('pe', lambda e: e.matmul(ps[pg][:, :], lhsT=UPB[j][:, 2, :], rhs=LO[2][:, sl], start=True, stop=False), reads=[f"UPB{j}", 'LO2'], writes=[f"ps{pg}"])
                    P.op('pe', lambda e: e.matmul(ps[pg][:, :], lhsT=UPB[j][:, 3, :], rhs=LO[3][:, sl], start=False, stop=True), reads=[f"UPB{j}", 'LO3'], writes=[f"ps{pg}"])
                    P.op('act', lambda e: e.activation(out=Ft["E1"][:, sl], in_=ps[pw][:, :], func=AF.Exp, bias=c(0), scale=-1.0), reads=[f"ps{pw}", 'COL'], writes=["E1"])
                    P.op('act', lambda e: e.activation(out=Ft["A"][:, sl], in_=ps[pa][:, :], func=AF.Sigmoid, bias=c(1), scale=1.0), reads=[f"ps{pa}", 'COL'], writes=["A"])
                    P.op('act', lambda e: e.activation(out=Ft["G"][:, sl], in_=ps[pg][:, :], func=AF.Copy), reads=[f"ps{pg}"], writes=["G"])
                    if layer1:
                        pv = nps()
                        P.op('pe', lambda e: e.matmul(ps[pv][:, :], lhsT=UPB[j][:, 4, :], rhs=LO[4][:, sl], start=True, stop=True), reads=[f"UPB{j}", 'LO4'], writes=[f"ps{pv}"])
                        P.op('act', lambda e: e.activation(out=Ft["T2"][:, sl], in_=ps[pv][:, :], func=AF.Sigmoid, bias=c(6), scale=1.0), reads=[f"ps{pv}", 'COL'], writes=["T2"])
                if layer1:
                    P.dma('act', Ft["T1"][:], vfT[j * 128:(j + 1) * 128, tsl], writes=["T1"])
                    P.op('pool', lambda e: e.tensor_tensor(out=Ft["T1"][:], in0=Ft["T1"][:], in1=Ft["V"][:], op=ALU.subtract), reads=["T1", "V"], writes=["T1"])
                    P.op('dve', lambda e: e.tensor_tensor(out=Ft["T1"][:], in0=Ft["T1"][:], in1=Ft["T2"][:], op=ALU.mult), reads=["T1", "T2"], writes=["T1"])
                    P.op('dve', lambda e: e.tensor_tensor(out=Ft["V"][:], in0=Ft["V"][:], in1=Ft["T1"][:], op=ALU.add), reads=["V", "T1"], writes=["V"])
                P.op('dve', lambda e: e.tensor_scalar(out=Ft["E1"][:], in0=Ft["E1"][:], scalar1=1.0, scalar2=None, op0=ALU.add), reads=["E1"], writes=["E1"])
                P.op('dve', lambda e: e.reciprocal(out=Ft["E1"][:], in_=Ft["E1"][:]), reads=["E1"], writes=["E1"])
                P.op('act', lambda e: e.activation(out=Ft["Wd"][:], in_=Ft["E1"][:], func=AF.Exp, scale=-float(np.exp(-0.5))), reads=["E1"], writes=["Wd"])
                P.op('dve', lambda e: e.tensor_scalar(out=Ft["KK"][:], in0=Ft["K"][:], scalar1=c(2), scalar2=None, op0=ALU.mult), reads=["K", 'COL'], writes=["KK"])
                P.op('pool', lambda e: e.tensor_tensor(out=Ft["T1"][:], in0=Ft["KK"][:], in1=Ft["KK"][:], op=ALU.mult), reads=["KK"], writes=["T1"])
                for b, pb in enumerate(bsum(Ft["T1"], "T1")):
                    sl = slice(b * 512, (b + 1) * 512)
                    P.op('dve', lambda e: e.tensor_scalar(out=Ft["T2"][:, sl], in0=ps[pb][:, :], scalar1=1e-18, scalar2=None, op0=ALU.max), reads=[f"ps{pb}"], writes=["T2"])
                P.op('act', lambda e: e.activation(out=Ft["T2"][:], in_=Ft["T2"][:], func=AF.Ln), reads=["T2"], writes=["T2"])
                P.op('act', lambda e: e.activation(out=Ft["T2"][:], in_=Ft["T2"][:], func=AF.Exp, scale=-0.5), reads=["T2"], writes=["T2"])
                P.op('dve', lambda e: e.tensor_tensor(out=Ft["KK"][:], in0=Ft["KK"][:], in1=Ft["T2"][:], op=ALU.mult), reads=["KK", "T2"], writes=["KK"])
                P.op('pool', lambda e: e.tensor_scalar(out=Ft["AA"][:], in0=Ft["KK"][:], scalar1=-1.0, scalar2=None, op0=ALU.mult), reads=["KK"], writes=["AA"])
                P.op('dve', lambda e: e.tensor_tensor(out=Ft["BBv"][:], in0=Ft["KK"][:], in1=Ft["A"][:], op=ALU.mult), reads=["KK", "A"], writes=["BBv"])
                P.op('dve', lambda e: e.tensor_scalar(out=Ft["T1"][:], in0=Ft["A"][:], scalar1=c(3), scalar2=c(4), op0=ALU.mult, op1=ALU.add), reads=["A", 'COL'], writes=["T1"])
                P.op('dve', lambda e: e.tensor_tensor(out=Ft["Kp"][:], in0=Ft["K"][:], in1=Ft["T1"][:], op=ALU.mult), reads=["K", "T1"], writes=["Kp"])
                P.op('pool', lambda e: e.tensor_tensor(out=Ft["T1"][:], in0=Ft["BBv"][:], in1=Ft["R"][:], op=ALU.mult), reads=["BBv", "R"], writes=["T1"])
                P.op('dve', lambda e: e.tensor_tensor(out=Ft["Rp"][:], in0=Ft["Wd"][:], in1=Ft["R"][:], op=ALU.mult), reads=["Wd", "R"], writes=["Rp"])
                for b, pb in enumerate(bsum(Ft["T1"], "T1")):
                    sl = slice(b * 512, (b + 1) * 512)
                    P.op('dve', lambda e: e.tensor_tensor(out=Ft["T2"][:, sl], in0=Ft["AA"][:, sl], in1=ps[pb][:, :], op=ALU.mult), reads=["AA", f"ps{pb}"], writes=["T2"])
                P.op('dve', lambda e: e.tensor_tensor(out=Ft["Rp"][:], in0=Ft["Rp"][:], in1=Ft["T2"][:], op=ALU.add), reads=["Rp", "T2"], writes=["Rp"])
                P.op('pool', lambda e: e.tensor_tensor(out=Ft["T1"][:], in0=Ft["R"][:], in1=Ft["Kp"][:], op=ALU.mult), reads=["R", "Kp"], writes=["T1"])
                for b, pb in enumerate(bsum(Ft["T1"], "T1")):
                    sl = slice(b * 512, (b + 1) * 512)
                    P.op('act', lambda e: e.activation(out=Ft["RK"][:, sl], in_=ps[pb][:, :], func=AF.Copy), reads=[f"ps{pb}"], writes=["RK"])
                P.op('dve', lambda e: e.tensor_scalar(out=Ft["T2"][:], in0=Ft["T1"][:], scalar1=c(5), scalar2=None, op0=ALU.mult), reads=["T1", 'COL'], writes=["T2"])
                for b, pb in enumerate(bsum(Ft["T2"], "T2")):
                    sl = slice(b * 512, (b + 1) * 512)
                    P.op('dve', lambda e: e.tensor_tensor(out=Ft["BON"][:, sl], in0=Ft["V"][:, sl], in1=ps[pb][:, :], op=ALU.mult), reads=["V", f"ps{pb}"], writes=["BON"])
                for sub in range(8):
                    ssl = slice(sub * 128, (sub + 1) * 128)
                    for ai, nn in enumerate(["AA", "BBv", "Kp", "V", "RK"]):
                        pb = nps()
                        P.op('pe', lambda e: e.transpose(out=ps[pb][:, 0:128], in_=Ft[nn][:, ssl], identity=IDENT),
                             reads=[nn, 'CONST'], writes=[f"ps{pb}"])
                        src3 = ps[pb][:, 0:128].rearrange("p (h k) -> p h k", k=64)
                        if nn == "AA":
                            dst, dk = TMA[:, sub, :, :], "TMA"
                        elif nn == "BBv":
                            dst, dk = TMB[:, sub, :, :], "TMB"
                        elif nn == "Kp":
                            dst, dk = TMK[:, sub, :, 0:64], "TMK"
                        elif nn == "V":
                            dst, dk = TMK[:, sub, :, 65:129], "TMK"
                        else:
                            dst, dk, src3 = TMK[:, sub, :, 64:65], "TMK", src3[:, :, 0:1]
                        if ai % 2 == 0:
                            P.op('act', lambda e: e.activation(out=dst, in_=src3, func=AF.Copy), reads=[f"ps{pb}"], writes=[dk])
                        else:
                            P.op('dve', lambda e: e.tensor_copy(out=dst, in_=src3), reads=[f"ps{pb}"], writes=[dk])
                hs = slice(2 * j, 2 * j + 2)
                P.dma('sp', ATM[tsl, hs, :].rearrange("(s p) h k -> p s h k", p=128), TMA[:], reads=["TMA"], writes=["ATMd"])
                P.dma('sp', BTM[tsl, hs, :].rearrange("(s p) h k -> p s h k", p=128), TMB[:], reads=["TMB"], writes=["BTMd"])
                for hh in range(2):
                    P.dma('sp', KRV[2 * j + hh, tsl, :].rearrange("(s p) n -> p s n", p=128), TMK[:, :, hh, :], reads=["TMK"], writes=["KRVd"])
                for hh in range(2):
                    P.dma('pool', RPT[:, 2 * j + hh, tsl], Ft["Rp"][hh * 64:(hh + 1) * 64, :], reads=["Rp"], writes=["RPTd"])
                    P.dma('pool', WTS[:, 2 * j + hh, tsl], Ft["Wd"][hh * 64:(hh + 1) * 64, :], reads=["Wd"], writes=["WTSd"])
                P.dma('pool', BONT[j * 128:(j + 1) * 128, tsl], Ft["BON"][:], reads=["BON"], writes=["BONTd"])
                P.dma('pool', GT[j * 128:(j + 1) * 128, tsl], Ft["G"][:], reads=["G"], writes=["GTd"])
                P.dma('act', vT[j * 128:(j + 1) * 128, tsl], Ft["V"][:], reads=["V"], is_output=True)
    with P.scope():
        S = [[P.sb(f"S{h}_{p}", [97, CH + 1, 64], BF16) for p in range(2)] for h in range(HPC)]
        LT = [[P.sb(f"LT{h}_{p}", [97, CH, 65], BF16) for p in range(2)] for h in range(HPC)]
        A8 = [P.sb(f"A8_{p}", [8, G, HPC, 64], BF16) for p in range(2)]
        BB = [P.sb(f"BB_{p}", [8, G * HPC, 8, 64], BF16) for p in range(2)]
        RP = [P.sb(f"RP_{p}", [64, HPC, CH], F32) for p in range(2)]
        WT = [P.sb(f"WT_{p}", [65, HPC, CH], F32) for p in range(2)]
        psS = [P.ps(f"psS{h}", [128, 512]) for h in range(HPC)]
        psO = [P.ps(f"psO{i}", [128, 512]) for i in range(2)]
        NCH = T // CH
        for p in range(2):
            for h in range(HPC):
                P.op('dve', lambda e: e.memset(S[h][p][:], 0.0), writes=[f"S{h}_{p}"])
                P.op('pool', lambda e: e.memset(LT[h][p][:], 0.0), writes=[f"LT{h}_{p}"])
            P.op('dve', lambda e: e.memset(WT[p][:], 0.0), writes=[f"WT_{p}"])
            P.op('pool', lambda e: e.memset(BB[p][:], 0.0), writes=[f"BB_{p}"])

        def prep(c):
            p = c % 2
            t0 = c * CH
            a8v = ATM[t0:t0 + CH, :, :].rearrange("(g p) h k -> p g h k", p=8)
            b8v = BTM[t0:t0 + CH, :, :].rearrange("(g p) h k -> p g h k", p=8)
            P.dma('sp', A8[p][:], a8v, reads=["ATMd"], writes=[f"A8_{p}"]); yield 'd'
            P.dma('sp', RP[p][:], RPT[:, :, t0:t0 + CH], reads=["RPTd"], writes=[f"RP_{p}"]); yield 'd'
            P.dma('sp', WT[p][0:64, :, :], WTS[:, :, t0:t0 + CH], reads=["WTSd"], writes=[f"WT_{p}"]); yield 'd'
            for j in range(8):
                P.dma('sp', BB[p][j:j + 1, :, j, :].rearrange("p (g h) k -> p g h k", h=HPC), b8v[j:j + 1], reads=["BTMd"], writes=[f"BB_{p}"]); yield 'd'
            for h in range(HPC):
                P.dma('sp', LT[h][p][96:97, :, :], KRV[h:h + 1, t0:t0 + CH, 0:65], reads=["KRVd"], writes=[f"LT{h}_{p}"]); yield 'd'
                P.dma('sp', S[h][p][96:97, 0:CH, :], KRV[h:h + 1, t0:t0 + CH, 65:129], reads=["KRVd"], writes=[f"S{h}_{p}"]); yield 'd'
            for h in range(HPC):
                P.op('pool', lambda e: e.tensor_copy(out=LT[h][p][0:64, :, 64], in_=RP[p][:, h, :]), reads=[f"RP_{p}"], writes=[f"LT{h}_{p}"]); yield 'p'
            for h in range(HPC):
                for g in range(G):
                    o = (h * G + g) % 2
                    P.op('pe', lambda e: e.matmul(psO[o][0:64, 0:512], lhsT=A8[p][:, g, h, :],
                                                  rhs=BB[p][:, g * HPC + h, :, :].rearrange("p j k -> p (j k)"), start=True, stop=True),
                         reads=[f"A8_{p}", f"BB_{p}"], writes=[f"psO{o}"])
                    P.op('act', lambda e: e.activation(out=LT[h][p][0:64, g * 8:(g + 1) * 8, 0:64],
                                                       in_=psO[o][0:64, 0:512].rearrange("p (j k) -> p j k", k=64), func=AF.Copy),
                         reads=[f"psO{o}"], writes=[f"LT{h}_{p}"]); yield 'm'

        def steps(c, gen):
            p = c % 2
            pending = gen is not None
            for tl in range(CH):
                if pending:
                    n_ = 23 if tl == 0 else (-(-(HPC * G) // (CH - CH // 2)) if tl >= CH // 2 else 0)
                    for _ in range(n_):
                        try:
                            next(gen)
                        except StopIteration:
                            pending = False
                            break
                for h in range(HPC):
                    col = (tl % 8) * 64
                    P.op('pe', lambda e: e.matmul(psS[h][0:65, col:col + 64], lhsT=LT[h][p][:, tl, :], rhs=S[h][p][:, tl, :], start=True, stop=True),
                         reads=[f"LT{h}_{p}", f"S{h}_{p}"], writes=[f"psS{h}"])
                    P.op('dve', lambda e: e.scalar_tensor_tensor(out=S[h][p][0:65, tl + 1, :], in0=S[h][p][0:65, tl, :],
                                                                 scalar=WT[p][0:65, h, tl:tl + 1], in1=psS[h][0:65, col:col + 64],
                                                                 op0=ALU.mult, op1=ALU.add),
                         reads=[f"S{h}_{p}", f"WT_{p}", f"psS{h}"], writes=[f"S{h}_{p}"])
            if gen is not None:
                for _ in gen:
                    pass
            t0 = c * CH
            for h in range(HPC):
                P.op('pool', lambda e: e.tensor_copy(out=S[h][1 - p][0:65, 0, :], in_=S[h][p][0:65, CH, :]), reads=[f"S{h}_{p}"], writes=[f"S{h}_{1 - p}"])
                P.dma('pool', YTM[h:h + 1, t0:t0 + CH, :], S[h][p][64:65, 1:CH + 1, :], reads=[f"S{h}_{p}"], writes=["YTMd"])

        for _ in prep(0):
            pass
        for c in range(NCH):
            steps(c, prep(c + 1) if c + 1 < NCH else None)
    with P.scope():
        YT = P.sb("YT", [128, 8, 2, 64], BF16)
        Y = [P.sb(f"Y{i}", [128, TB], F32) for i in range(2)]
        B = [P.sb(f"B{i}", [128, TB], F32) for i in range(2)]
        Gg = [P.sb(f"Gg{i}", [128, TB], F32) for i in range(2)]
        SQ = P.sb("SQ", [128, TB], F32); MU = P.sb("MU", [128, TB], F32); RS = P.sb("RS", [128, TB], F32)
        LNC = P.sb("LNC", [128, 2, 2], F32); EPSG = P.sb("EPSG", [128, 1], F32)
        psT = [P.ps(f"psT{i}", [128, 1024], BF16) for i in range(2)]
        psg = [P.ps(f"psg{i}", [128, 512]) for i in range(4)]
        P.dma('sp', LNC[:], lncols[:, :, :], writes=['LNC'])
        P.op('dve', lambda e: e.memset(EPSG[:], GN_EPS), writes=['EPSG'])
        for tb in range(NBLK):
            tsl = slice(tb * TB, (tb + 1) * TB)
            for j in range(2):
                i = j
                hs = slice(2 * j, 2 * j + 2)
                for hh in range(2):
                    P.dma('sp', YT[:, :, hh, :], YTM[2 * j + hh, tsl, :].rearrange("(s p) k -> p s k", p=128), reads=["YTMd"], writes=["YT"])
                P.dma('act', B[i][:], BONT[j * 128:(j + 1) * 128, tsl], reads=["BONTd"], writes=[f"B{i}"])
                P.dma('sp', Gg[i][:], GT[j * 128:(j + 1) * 128, tsl], reads=["GTd"], writes=[f"Gg{i}"])
                for sub in range(8):
                    P.op('pe', lambda e: e.transpose(out=psT[i][:, sub * 128:(sub + 1) * 128], in_=YT[:, sub, :, :].rearrange("p h k -> p (h k)"), identity=IDB[:]),
                         reads=["YT", 'IDB'], writes=[f"psT{i}"])
                P.op('act', lambda e: e.activation(out=Y[i][:], in_=psT[i][:, :], func=AF.Copy), reads=[f"psT{i}"], writes=[f"Y{i}"])
                P.op('act', lambda e: e.activation(out=SQ[:], in_=Y[i][:], func=AF.Square), reads=[f"Y{i}"], writes=['SQ'])
                for b in range(2):
                    sl = slice(b * 512, (b + 1) * 512)
                    pm, pv = 2 * b, 2 * b + 1
                    P.op('pe', lambda e: e.matmul(psg[pm][:, :], lhsT=BONES64, rhs=Y[i][:, sl], start=True, stop=True), reads=['CONST', f"Y{i}"], writes=[f"psg{pm}"])
                    P.op('pe', lambda e: e.matmul(psg[pv][:, :], lhsT=BONES64, rhs=SQ[:, sl], start=True, stop=True), reads=['CONST', 'SQ'], writes=[f"psg{pv}"])
                    P.op('act', lambda e: e.activation(out=MU[:, sl], in_=psg[pm][:, :], func=AF.Copy), reads=[f"psg{pm}"], writes=['MU'])
                    P.op('dve', lambda e: e.tensor_tensor(out=RS[:, sl], in0=MU[:, sl], in1=MU[:, sl], op=ALU.mult), reads=['MU'], writes=['RS'])
                    P.op('dve', lambda e: e.tensor_tensor(out=RS[:, sl], in0=psg[pv][:, :], in1=RS[:, sl], op=ALU.subtract), reads=[f"psg{pv}", 'RS'], writes=['RS'])
                P.op('act', lambda e: e.activation(out=RS[:], in_=RS[:], func=AF.Ln, bias=EPSG[:, 0:1], scale=1.0), reads=['RS', 'EPSG'], writes=['RS'])
                P.op('act', lambda e: e.activation(out=RS[:], in_=RS[:], func=AF.Exp, scale=-0.5), reads=['RS'], writes=['RS'])
                P.op('dve', lambda e: e.tensor_tensor(out=Y[i][:], in0=Y[i][:], in1=MU[:], op=ALU.subtract), reads=[f"Y{i}", 'MU'], writes=[f"Y{i}"])
                P.op('dve', lambda e: e.tensor_tensor(out=Y[i][:], in0=Y[i][:], in1=RS[:], op=ALU.mult), reads=[f"Y{i}", 'RS'], writes=[f"Y{i}"])
                P.op('dve', lambda e: e.tensor_scalar(out=Y[i][:], in0=Y[i][:], scalar1=LNC[:, 0, j:j + 1], scalar2=LNC[:, 1, j:j + 1],
                                                      op0=ALU.mult, op1=ALU.add), reads=[f"Y{i}", 'LNC'], writes=[f"Y{i}"])
                P.op('pool', lambda e: e.tensor_tensor(out=Y[i][:], in0=Y[i][:], in1=B[i][:], op=ALU.add), reads=[f"Y{i}", f"B{i}"], writes=[f"Y{i}"])
                P.op('pool', lambda e: e.tensor_tensor(out=Y[i][:], in0=Y[i][:], in1=Gg[i][:], op=ALU.mult), reads=[f"Y{i}", f"Gg{i}"], writes=[f"Y{i}"])
                P.dma('pool', yrwT[j * 128:(j + 1) * 128, tsl], Y[i][:], reads=[f"Y{i}"], is_output=True)
    P.finish('sp')
    print("L1 ninstr", P.ninstr, "nsem", P.nsem)
    es.close()
    return nc

POOL_WINDOWS = (2, 4, 8, 16)

def l1_consts():
    bo = np.zeros((128, 128), np.float32); bo[:64, :64] = 1; bo[64:, 64:] = 1
    return np.stack([np.ones((128, 128), np.float32), bo, bo / 64, np.eye(128, dtype=np.float32)], 0)

def l1_core_inputs(c, xT, L, w):
    f32 = np.float32
    g, half = c // 2, c % 2
    W = w['w_in'][L]
    base = 3072
    chs = [256 * c + j * 128 + np.arange(128) for j in range(2)]
    colsets = [g * 256 + np.arange(128), g * 256 + 128 + np.arange(128),
               1024 + c * 128 + np.arange(128), 2048 + c * 128 + np.arange(128)]
    tiles = [np.ascontiguousarray(W[:, cs]) for cs in colsets]
    def padcols(a):
        return np.concatenate([a, np.zeros((a.shape[0], 128 - a.shape[1]), f32)], 1)
    tiles.append(padcols(W[:, base + 6144:base + 6240])); tiles.append(padcols(W[:, base + 6240:base + 6336]))
    tiles.append(W[:, base + 6336:base + 6464]); tiles.append(W[:, base + 6464:base + 6592])
    tiles.append(padcols(w['rwkv_v_down'][L - 1]) if L > 0 else np.zeros((4096, 128), f32))
    for j in range(2):
        for off in (0, 2048, 4096):
            tiles.append(W[:, base + off + chs[j]])
    wt = np.stack([t.reshape(32, 128, 128).transpose(1, 0, 2) for t in tiles], 0)
    sm = w['shift_mix'][L]
    mixc = np.zeros((128, 11), f32)
    for j in range(2):
        for i, off in enumerate((0, 2048, 4096)):
            mixc[:, 3 * j + i] = sm[off + chs[j]]
    mixc[:96, 6] = sm[6144:6240]; mixc[:96, 7] = sm[6240:6336]; mixc[:, 8] = sm[6336:6464]; mixc[:, 9] = sm[6464:6592]
    if L > 0:
        mixc[:64, 10] = w['rwkv_v_shift'][L - 1]
    cols = np.zeros((128, 8, 2), f32)
    rk = w['rwkv_r_k'][L].reshape(-1)
    for j in range(2):
        ch = chs[j]
        cols[:, 0, j] = w['rwkv_w0'][L][ch]; cols[:, 1, j] = w['rwkv_a0'][L][ch]; cols[:, 2, j] = w['rwkv_k_k'][L][ch]
        cols[:, 3, j] = w['rwkv_k_a'][L][ch]; cols[:, 5, j] = rk[ch]
        if L > 0:
            cols[:, 6, j] = w['rwkv_v0'][L - 1][ch]
    ups = np.zeros((2, 128, 5, 128), f32)
    for j in range(2):
        ch = chs[j]
        ups[j, :96, 0] = w['rwkv_w_up'][L][:, ch]; ups[j, :96, 1] = w['rwkv_a_up'][L][:, ch]
        ups[j, :, 2] = w['rwkv_g_up'][L][:128, ch]; ups[j, :, 3] = w['rwkv_g_up'][L][128:, ch]
        if L > 0:
            ups[j, :64, 4] = w['rwkv_v_up'][L - 1][:, ch]
    lncols = np.stack([np.stack([w['rwkv_ln_g'][L][chs[j]] for j in range(2)], 1),
                       np.stack([w['rwkv_ln_b'][L][chs[j]] for j in range(2)], 1)], 1)
    pw = w['pool_w'][L][g]
    pwt = np.ascontiguousarray(pw[:, half * 128:(half + 1) * 128].reshape(2, 128, 128).transpose(1, 0, 2))
    pcol = np.zeros((128, 5), f32); pcol[:, g] = 1.0 / POOL_WINDOWS[g]
    pcol[:, 4] = w['pool_scale'][L][g * 256 + half * 128 + np.arange(128)]
    pinv = np.zeros((128, 4, 16), f32); pinv[:, g, :] = 1.0 / np.minimum(np.arange(16) + 1, POOL_WINDOWS[g])
    dwT = np.zeros((128, KW + 1), f32)
    dwT[:, :KW] = w['conv_dw'][L][:, c * 128:(c + 1) * 128].T; dwT[:, KW] = w['conv_dw_b'][L][c * 128:(c + 1) * 128]
    m = {"xT": xT, "gain": np.ascontiguousarray(w['mix_norm'][L].reshape(32, 128).T), "wt": wt, "consts": l1_consts(),
         "pwt": pwt, "pcol": pcol, "pinv": pinv, "dwT": dwT, "mixc": mixc, "cols": cols, "ups": ups, "lncols": lncols}
    return {k: np.ascontiguousarray(v, dtype=f32) for k, v in m.items()}

I32 = mybir.dt.int32
AX = mybir.AxisListType.X
NE = 64
EPS = 1e-6
LN_EPS = 1e-5


def n_moe_blocks(T):
    return -(-(2 * T + NE * 127) // 128)


def build_l3(T, final, mode='full'):
    nc = bass.Bass("TRN2", target_bir_lowering=False)
    NBLK = T // TB
    NT = T // 128
    NB = n_moe_blocks(T)
    din = lambda n, s, dt=F32: nc.dram_tensor(n, s, dt, kind="ExternalInput").ap()
    dscr = lambda n, s, dt=F32: nc.dram_tensor(n, s, dt).ap()
    FRONT, BACK = mode in ('full', 'front'), mode in ('full', 'back')
    dout = lambda n, s, dt=F32: nc.dram_tensor(n, s, dt, kind="ExternalOutput").ap()
    consts = din("consts", [3, 128, 128])
    grows = din("grows", [2, D])
    if FRONT:
        xT = din("xT", [D, T]); mixT = din("mixT", [D, T])
        cvcols = din("cvcols", [128, 2, 8]); pwt = din("pwt", [8, 128, 8, 128])
        wot = din("wot", [KC, 128, KC, 128])
        fgain = din("fgain", [128, KC])
        wr = din("wr", [128, KC, 72]); rbias = din("rbias", [1, 72])
        YCONV = dscr("YCONV", [1024, T]); X1T = dscr("X1T", [D, T])
    if BACK:
        ewin = din("ewin", [NE * 128 * 4, 8192]); ewout = din("ewout", [NE * 128 * 2, 8192])
        bgrid = din("bgrid", [64, NB]); iotap = din("iotap", [128, 1])
        tokrow = din("tokrow", [128, NT, 16], I32)
        out = dout("out", [T, D])
    if mode == 'full':
        X1 = dscr("X1", [T, D]); HN = dscr("HN", [T + 128, D])
    elif mode == 'front':
        X1 = dout("x1_out", [T, D]); HN = dout("hn_out", [T, D])
        mk_out = dout("mk_out", [128, NT, 2, 64]); gates_out = dout("gates_out", [128, NT, 2])
    else:
        X1 = din("x1_in", [T, D]); HN = din("hn_in", [T + 128, D])
        mk_in = din("mk_in", [128, NT, 2, 64]); gates_in = din("gates_in", [128, NT, 2])
    if BACK:
        SLOTTOK = dscr("SLOTTOK", [NB * 128, 16], I32); YS = dscr("YS", [NB * 128, D], BF16)
    es = ExitStack(); P = Prog(nc, es)
    CONST = P.sb("CONST", [128, 3, 128], F32)
    P.dma('sp', CONST[:], consts.rearrange("c p n -> p c n"), writes=['CONST'])
    ONES, IDENT, TRI = CONST[:, 0, :], CONST[:, 1, :], CONST[:, 2, :]
    GATES = P.sb("GATES", [128, NT, 2], F32)
    DESTI = P.sb("DESTI", [128, NT, 2], I32)
    WINI = P.sb("WINI", [128, NB, 4], I32); WOUTI = P.sb("WOUTI", [128, NB, 2], I32)
    GBC = P.sb("GBC", [128, D], F32)

    def bcast_row(dst, dkey, row_ap, width, psl, rowt):
        P.dma('sp', rowt[0:1, 0:width], row_ap, writes=['ROWT'])
        for b in range(0, width, 512):
            wdt = min(512, width - b)
            P.op('pe', lambda e: e.matmul(psl[:, 0:wdt], lhsT=ONES[0:1, :], rhs=rowt[0:1, b:b + wdt], start=True, stop=True),
                 reads=['CONST', 'ROWT'], writes=['psl'])
            P.op('act', lambda e: e.activation(out=dst[:, b:b + wdt], in_=psl[:, 0:wdt], func=AF.Copy), reads=['psl'], writes=[dkey])

    if FRONT:
        with P.scope():
            HC = [P.sb(f"HC{j}", [128, TB], F32) for j in range(8)]
            SQ = [P.sb(f"SQ{i}", [128, TB], F32) for i in range(2)]
            MEAN = P.sb("MEAN", [128, TB], F32); RSTD = P.sb("RSTD", [128, TB], F32); M2 = P.sb("M2", [128, TB], F32)
            AC = [P.sb(f"AC{j}", [128, TB], BF16) for j in range(8)]
            PWF = [P.sb(f"PWF{i}", [128, 8, 128], F32) for i in range(2)]
            PWB = [P.sb(f"PWB{i}", [128, 8, 128], BF16) for i in range(2)]
            OT = [P.sb(f"OT{i}", [128, TB], F32) for i in range(2)]
            COL = P.sb("COL", [128, 2, 8], F32); EPSC = P.sb("EPSC", [128, 1], F32)
            psM = [P.ps(f"psM{b}", [128, 512]) for b in range(2)]
            psV = [P.ps(f"psV{b}", [128, 512]) for b in range(2)]
            psO = [P.ps(f"psO{i}", [128, 512]) for i in range(2)]
            P.dma('sp', COL[:], cvcols[:, :, :], writes=['COL'])
            P.op('dve', lambda e: e.memset(EPSC[:], LN_EPS), writes=['EPSC'])
            for tb in range(NBLK):
                tsl = slice(tb * TB, (tb + 1) * TB)
                for j in range(8):
                    i = j % 2
                    P.dma('sp' if i == 0 else 'act', HC[j][:], mixT[1024 + j * 128:1024 + (j + 1) * 128, tsl], writes=[f"HC{j}"])
                    P.op('act', lambda e: e.activation(out=SQ[i][:], in_=HC[j][:], func=AF.Square), reads=[f"HC{j}"], writes=[f"SQ{i}"])
                    for b in range(2):
                        sl = slice(b * 512, (b + 1) * 512)
                        P.op('pe', lambda e: e.matmul(psM[b][:, :], lhsT=ONES, rhs=HC[j][:, sl], start=(j == 0), stop=(j == 7)), reads=['CONST', f"HC{j}"], writes=[f"psM{b}"])
                        P.op('pe', lambda e: e.matmul(psV[b][:, :], lhsT=ONES, rhs=SQ[i][:, sl], start=(j == 0), stop=(j == 7)), reads=['CONST', f"SQ{i}"], writes=[f"psV{b}"])
                for b in range(2):
                    sl = slice(b * 512, (b + 1) * 512)
                    P.op('act', lambda e: e.activation(out=MEAN[:, sl], in_=psM[b][:, :], func=AF.Copy, scale=1.0 / 1024), reads=[f"psM{b}"], writes=['MEAN'])
                    P.op('dve', lambda e: e.tensor_tensor(out=M2[:, sl], in0=MEAN[:, sl], in1=MEAN[:, sl], op=ALU.mult), reads=['MEAN'], writes=['M2'])
                    P.op('dve', lambda e: e.scalar_tensor_tensor(out=RSTD[:, sl], in0=psV[b][:, :], scalar=1.0 / 1024, in1=M2[:, sl],
                                                                 op0=ALU.mult, op1=ALU.subtract), reads=[f"psV{b}", 'M2'], writes=['RSTD'])
                P.op('act', lambda e: e.activation(out=RSTD[:], in_=RSTD[:], func=AF.Ln, bias=EPSC[:, 0:1], scale=1.0), reads=['RSTD', 'EPSC'], writes=['RSTD'])
                P.op('act', lambda e: e.activation(out=RSTD[:], in_=RSTD[:], func=AF.Exp, scale=-0.5), reads=['RSTD'], writes=['RSTD'])
                for j in range(8):
                    eng = 'dve' if j % 2 == 0 else 'pool'
                    P.op(eng, lambda e: e.tensor_tensor(out=HC[j][:], in0=HC[j][:], in1=MEAN[:], op=ALU.subtract), reads=[f"HC{j}", 'MEAN'], writes=[f"HC{j}"])
                    P.op(eng, lambda e: e.tensor_tensor(out=HC[j][:], in0=HC[j][:], in1=RSTD[:], op=ALU.mult), reads=[f"HC{j}", 'RSTD'], writes=[f"HC{j}"])
                    P.op('act', lambda e: e.activation(out=AC[j][:], in_=HC[j][:], func=AF.Silu, bias=COL[:, 1, j:j + 1], scale=COL[:, 0, j:j + 1]),
                         reads=[f"HC{j}", 'COL'], writes=[f"AC{j}"])
                ack = [f"AC{j}" for j in range(8)]
                for dt in range(8):
                    i = dt % 2
                    P.dma('sp', PWF[i][:], pwt[dt], writes=[f"PWF{i}"])
                    P.op('pool', lambda e: e.tensor_copy(out=PWB[i][:], in_=PWF[i][:]), reads=[f"PWF{i}"], writes=[f"PWB{i}"])
                    for b in range(2):
                        pb = b
                        for kc in range(8):
                            P.op('pe', lambda e: e.matmul(psO[pb][:, :], lhsT=PWB[i][:, kc, :], rhs=AC[kc][:, b * 512:(b + 1) * 512], start=(kc == 0), stop=(kc == 7)),
                                 reads=[f"PWB{i}"] + (ack if dt == 0 and b == 0 else []), writes=[f"psO{pb}"])
                        P.op('act', lambda e: e.activation(out=OT[i][:, b * 512:(b + 1) * 512], in_=psO[pb][:, :], func=AF.Copy), reads=[f"psO{pb}"], writes=[f"OT{i}"])
                    P._commit((P.cur['pe'][0], P.cur['pe'][1]), ack + [f"PWB{i}"], [])
                    P.dma('pool', YCONV[dt * 128:(dt + 1) * 128, tsl], OT[i][:], reads=[f"OT{i}"], writes=["YCd"])
        with P.scope():
            MB = P.sb("MB", [128, KC, TB], BF16)
            MF = [P.sb(f"MF{i}", [128, TB], F32) for i in range(2)]
            WF = [P.sb(f"WF{i}", [128, KC, 128], F32) for i in range(2)]
            WB = [P.sb(f"WB{i}", [128, KC, 128], BF16) for i in range(2)]
            XI = [P.sb(f"XI{i}", [128, TB], F32) for i in range(2)]
            psB = [P.ps(f"psB{i}", [128, 512]) for i in range(4)]
            for tb in range(NBLK):
                tsl = slice(tb * TB, (tb + 1) * TB)
                for kc in range(KC):
                    i = kc % 2
                    src = YCONV[(kc - 8) * 128:(kc - 7) * 128, tsl] if 8 <= kc < 16 else mixT[kc * 128:(kc + 1) * 128, tsl]
                    P.dma('sp' if i == 0 else 'act', MF[i][:], src, writes=[f"MF{i}"])
                    if kc % 3 == 0:
                        P.op('dve', lambda e: e.tensor_copy(out=MB[:, kc, :], in_=MF[i][:]), reads=[f"MF{i}"], writes=[f"MB{kc}"])
                    elif kc % 3 == 1:
                        P.op('pool', lambda e: e.tensor_copy(out=MB[:, kc, :], in_=MF[i][:]), reads=[f"MF{i}"], writes=[f"MB{kc}"])
                    else:
                        P.op('act', lambda e: e.activation(out=MB[:, kc, :], in_=MF[i][:], func=AF.Copy), reads=[f"MF{i}"], writes=[f"MB{kc}"])
                mbk = [f"MB{kc}" for kc in range(KC)]
                for dt in range(KC):
                    i = dt % 2
                    P.dma('sp', WF[i][:], wot[dt], writes=[f"WF{i}"])
                    P.op('pool', lambda e: e.tensor_copy(out=WB[i][:], in_=WF[i][:]), reads=[f"WF{i}"], writes=[f"WB{i}"])
                    P.dma('act', XI[i][:], xT[dt * 128:(dt + 1) * 128, tsl], writes=[f"XI{i}"])
                    for b in range(2):
                        pb = (dt * 2 + b) % 4
                        for kc in range(KC):
                            P.op('pe', lambda e: e.matmul(psB[pb][:, :], lhsT=WB[i][:, kc, :], rhs=MB[:, kc, b * 512:(b + 1) * 512], start=(kc == 0), stop=(kc == KC - 1)),
                                 reads=([f"WB{i}"] + (mbk if dt == 0 and b == 0 else [])) if kc == 0 else [], writes=[f"psB{pb}"])
                        P.op('dve', lambda e: e.tensor_tensor(out=XI[i][:, b * 512:(b + 1) * 512], in0=psB[pb][:, :], in1=XI[i][:, b * 512:(b + 1) * 512], op=ALU.add),
                             reads=[f"psB{pb}", f"XI{i}"], writes=[f"XI{i}"])
                    P._commit((P.cur['pe'][0], P.cur['pe'][1]), mbk + [f"WB{i}"], [])
                    P.dma('pool', X1T[dt * 128:(dt + 1) * 128, tsl], XI[i][:], reads=[f"XI{i}"], writes=["X1Td"])
    MKs = ExitStack()
    P.es = MKs
    MK = P.sb("MK", [128, NT, 2, 64], F32)
    RSEL = P.sb("RSEL", [128, NT, 2], F32)
    CARRY = P.sb("CARRY", [1, 64], F32)
    P.es = es
    if FRONT:
        with P.scope():
            X1C1 = P.sb("X1C0", [128, KC, 128], F32); X1C = [X1C1, X1C1]
            X1G = P.sb("X1G", [128, KC, 128], F32)
            X1R = [P.sb(f"X1R{i}", [128, D], F32) for i in range(2)]
            HNt1 = P.sb("HNt0", [128, D], F32); HNt = [HNt1, HNt1]
            JUNK = P.sb("JUNK", [128, D], BF16)
            FG = P.sb("FG", [128, KC], F32); WR = P.sb("WR", [128, KC, 72], F32); RBC = P.sb("RBC", [128, 72], F32)
            ROWT = P.sb("ROWT", [1, D], F32)
            SM = {n: P.sb("sm_" + n, [128, w_], F32) for n, w_ in
                  [("SS", 1), ("RS", 1), ("LG", 72), ("GMAX", 1), ("NG", 1), ("OHG", 8), ("EXG", 8), ("SE", 1), ("PG", 1), ("SEL", 8), ("M1", 1),
                   ("OH1", 8), ("SEL2", 8), ("M2", 1), ("OH2", 8), ("DD", 1), ("ED", 1), ("RDEN", 1), ("MSUM", 64), ("T64", 64), ("EPS", 1)]}
            psl = P.ps("psl", [128, 512]); psR = P.ps("psR", [128, 512]); psK = P.ps("psK", [128, 512]); psC = P.ps("psC", [128, 512])
            psT = [P.ps(f"psT{i}", [128, 512]) for i in range(2)]
            P.dma('sp', FG[:], fgain[:, :], writes=['FG']); P.dma('sp', WR[:], wr[:, :, :], writes=['WR'])
            bcast_row(GBC, 'GBC', grows[0:1, :], D, psl, ROWT)
            bcast_row(RBC, 'RBC', rbias[0:1, :], 72, psl, ROWT)
            P.op('dve', lambda e: e.memset(SM["EPS"][:], EPS), writes=['EPS'])
            P.op('dve', lambda e: e.memset(CARRY[:], 0.0), writes=['CARRY'])
            if mode == 'full':
                P.op('pool', lambda e: e.memset(HNt[1][:], 0.0), writes=["HNt0"])
                P.dma('sp', HN[T:T + 128, :], HNt[1][:], reads=["HNt0"], writes=["HNd"])
            s = lambda n: SM[n]
            for tt in range(NT):
                i = tt % 2
                tsl = slice(tt * 128, (tt + 1) * 128)
                P.dma('sp', X1C[i][:], X1T[:, tsl].rearrange("(kc p) t -> p kc t", p=128), reads=["X1Td"], writes=["X1C0"])
                for kc in range(KC):
                    P.op('dve' if kc % 2 == 0 else 'pool',
                         lambda e: e.tensor_scalar(out=X1G[:, kc, :], in0=X1C[i][:, kc, :], scalar1=FG[:, kc:kc + 1], scalar2=None, op0=ALU.mult),
                         reads=["X1C0", 'FG'], writes=[f"X1G{kc}"])
                for kc in range(KC):
                    P.op('pe', lambda e: e.matmul(psR[:, 0:72], lhsT=X1G[:, kc, :], rhs=WR[:, kc, :], start=(kc == 0), stop=(kc == KC - 1)),
                         reads=[f"X1G{kc}", 'WR'], writes=['psR'])
                for k4 in range(8):
                    pt = psT[k4 % 2]
                    for q in range(4):
                        kc = k4 * 4 + q
                        P.op('pe', lambda e: e.transpose(out=pt[:, q * 128:(q + 1) * 128], in_=X1C[i][:, kc, :], identity=IDENT),
                             reads=["X1C0", 'CONST'], writes=[f"psT{k4 % 2}"])
                    if k4 % 2 == 0:
                        P.op('act', lambda e: e.activation(out=X1R[i][:, k4 * 512:(k4 + 1) * 512], in_=pt[:, :], func=AF.Copy), reads=[f"psT{k4 % 2}"], writes=[f"X1R{i}"])
                    else:
                        P.op('dve', lambda e: e.tensor_copy(out=X1R[i][:, k4 * 512:(k4 + 1) * 512], in_=pt[:, :]), reads=[f"psT{k4 % 2}"], writes=[f"X1R{i}"])
                P.op('act', lambda e: e.activation(out=JUNK[:], in_=X1R[i][:], func=AF.Square, accum_out=s("SS")[:, 0:1]), reads=[f"X1R{i}"], writes=['JUNK', 'SS'])
                P.op('act', lambda e: e.activation(out=s("RS")[:], in_=s("SS")[:], func=AF.Ln, bias=s("EPS")[:, 0:1], scale=1.0 / D), reads=['SS', 'EPS'], writes=['RS'])
                P.op('act', lambda e: e.activation(out=s("RS")[:], in_=s("RS")[:], func=AF.Exp, scale=-0.5), reads=['RS'], writes=['RS'])
                P.op('dve', lambda e: e.scalar_tensor_tensor(out=HNt[i][:], in0=X1R[i][:], scalar=s("RS")[:, 0:1], in1=GBC[:], op0=ALU.mult, op1=ALU.mult),
                     reads=[f"X1R{i}", 'RS', 'GBC'], writes=["HNt0"])
                P.dma('pool', HN[tsl, :], HNt[i][:], reads=["HNt0"], writes=["HNd"], is_output=(mode == 'front'))
                P.dma('act', X1[tsl, :], X1R[i][:], reads=[f"X1R{i}"], writes=["X1d"], is_output=(mode == 'front'))
                P.op('dve', lambda e: e.scalar_tensor_tensor(out=s("LG")[:], in0=psR[:, 0:72], scalar=s("RS")[:, 0:1], in1=RBC[:], op0=ALU.mult, op1=ALU.add),
                     reads=['psR', 'RS', 'RBC'], writes=['LG'])
                LG = s("LG")
                P.op('dve', lambda e: e.reduce_max(out=s("GMAX")[:], in_=LG[:, 0:8], axis=AX), reads=['LG'], writes=['GMAX'])
                P.op('dve', lambda e: e.tensor_scalar(out=s("OHG")[:], in0=LG[:, 0:8], scalar1=s("GMAX")[:, 0:1], scalar2=None, op0=ALU.is_equal), reads=['LG', 'GMAX'], writes=['OHG'])
                P.op('dve', lambda e: e.tensor_scalar(out=s("NG")[:], in0=s("GMAX")[:], scalar1=-1.0, scalar2=None, op0=ALU.mult), reads=['GMAX'], writes=['NG'])
                P.op('act', lambda e: e.activation(out=s("EXG")[:], in_=LG[:, 0:8], func=AF.Exp, bias=s("NG")[:, 0:1], scale=1.0, accum_out=s("SE")[:, 0:1]),
                     reads=['LG', 'NG'], writes=['EXG', 'SE'])
                P.op('dve', lambda e: e.reciprocal(out=s("PG")[:], in_=s("SE")[:]), reads=['SE'], writes=['PG'])
                P.op('dve', lambda e: e.tensor_scalar(out=s("SEL")[:], in0=LG[:, 8:16], scalar1=s("OHG")[:, 0:1], scalar2=None, op0=ALU.mult), reads=['LG', 'OHG'], writes=['SEL'])
                for g in range(1, 8):
                    P.op('dve', lambda e: e.scalar_tensor_tensor(out=s("SEL")[:], in0=LG[:, 8 + 8 * g:16 + 8 * g], scalar=s("OHG")[:, g:g + 1], in1=s("SEL")[:],
                                                                 op0=ALU.mult, op1=ALU.add), reads=['LG', 'OHG', 'SEL'], writes=['SEL'])
                P.op('dve', lambda e: e.reduce_max(out=s("M1")[:], in_=s("SEL")[:], axis=AX), reads=['SEL'], writes=['M1'])
                P.op('dve', lambda e: e.tensor_scalar(out=s("OH1")[:], in0=s("SEL")[:], scalar1=s("M1")[:, 0:1], scalar2=None, op0=ALU.is_equal), reads=['SEL', 'M1'], writes=['OH1'])
                P.op('dve', lambda e: e.scalar_tensor_tensor(out=s("SEL2")[:], in0=s("OH1")[:], scalar=-1e30, in1=s("SEL")[:], op0=ALU.mult, op1=ALU.add),
                     reads=['OH1', 'SEL'], writes=['SEL2'])
                P.op('dve', lambda e: e.reduce_max(out=s("M2")[:], in_=s("SEL2")[:], axis=AX), reads=['SEL2'], writes=['M2'])
                P.op('dve', lambda e: e.tensor_scalar(out=s("OH2")[:], in0=s("SEL2")[:], scalar1=s("M2")[:, 0:1], scalar2=None, op0=ALU.is_equal), reads=['SEL2', 'M2'], writes=['OH2'])
                P.op('dve', lambda e: e.tensor_tensor(out=s("DD")[:], in0=s("M2")[:], in1=s("M1")[:], op=ALU.subtract), reads=['M1', 'M2'], writes=['DD'])
                P.op('act', lambda e: e.activation(out=s("ED")[:], in_=s("DD")[:], func=AF.Exp), reads=['DD'], writes=['ED'])
                P.op('dve', lambda e: e.tensor_scalar(out=s("ED")[:], in0=s("ED")[:], scalar1=1.0, scalar2=None, op0=ALU.add), reads=['ED'], writes=['ED'])
                P.op('dve', lambda e: e.reciprocal(out=s("RDEN")[:], in_=s("ED")[:]), reads=['ED'], writes=['RDEN'])
                P.op('dve', lambda e: e.tensor_tensor(out=GATES[:, tt, 0:1], in0=s("PG")[:], in1=s("RDEN")[:], op=ALU.mult), reads=['PG', 'RDEN'], writes=['GATES'])
                P.op('dve', lambda e: e.tensor_tensor(out=GATES[:, tt, 1:2], in0=s("PG")[:], in1=GATES[:, tt, 0:1], op=ALU.subtract), reads=['PG', 'GATES'], writes=['GATES'])
                for g in range(8):
                    P.op('pool', lambda e: e.tensor_scalar(out=MK[:, tt, 0, g * 8:(g + 1) * 8], in0=s("OH1")[:], scalar1=s("OHG")[:, g:g + 1], scalar2=None, op0=ALU.mult),
                         reads=['OH1', 'OHG'], writes=['MK'])
                    P.op('pool', lambda e: e.tensor_scalar(out=MK[:, tt, 1, g * 8:(g + 1) * 8], in0=s("OH2")[:], scalar1=s("OHG")[:, g:g + 1], scalar2=None, op0=ALU.mult),
                         reads=['OH2', 'OHG'], writes=['MK'])
                P.op('dve', lambda e: e.tensor_tensor(out=s("MSUM")[:], in0=MK[:, tt, 0, :], in1=MK[:, tt, 1, :], op=ALU.add), reads=['MK'], writes=['MSUM'])
                P.op('pe', lambda e: e.matmul(psK[:, 0:64], lhsT=TRI, rhs=s("MSUM")[:], start=True, stop=False), reads=['CONST', 'MSUM'], writes=['psK'])
                P.op('pe', lambda e: e.matmul(psK[:, 0:64], lhsT=ONES[0:1, :], rhs=CARRY[0:1, :], start=False, stop=True), reads=['CONST', 'CARRY'], writes=['psK'])
                for k in range(2):
                    P.op('dve', lambda e: e.tensor_tensor(out=s("T64")[:], in0=MK[:, tt, k, :], in1=psK[:, 0:64], op=ALU.mult), reads=['MK', 'psK'], writes=['T64'])
                    P.op('dve', lambda e: e.reduce_sum(out=RSEL[:, tt, k:k + 1], in_=s("T64")[:], axis=AX), reads=['T64'], writes=['RSEL'])
                P.op('pe', lambda e: e.matmul(psC[0:1, 0:64], lhsT=ONES[:, 0:1], rhs=s("MSUM")[:], start=True, stop=True), reads=['CONST', 'MSUM'], writes=['psC'])
                P.op('dve', lambda e: e.tensor_tensor(out=CARRY[0:1, :], in0=CARRY[0:1, :], in1=psC[0:1, 0:64], op=ALU.add), reads=['CARRY', 'psC'], writes=['CARRY'])
        if mode == 'front':
            P.dma('sp', mk_out[:, :, :, :], MK[:], reads=['MK'], is_output=True)
            P.dma('sp', gates_out[:, :, :], GATES[:], reads=['GATES'], is_output=True)
    else:
        P.dma('sp', MK[:], mk_in[:, :, :, :], writes=['MK'])
        P.dma('sp', GATES[:], gates_in[:, :, :], writes=['GATES'])
        with P.scope():
            MSUM = P.sb("MSUMb", [128, 64], F32); T64r = P.sb("T64r", [128, 64], F32)
            psK = P.ps("psKb", [128, 512]); psC = P.ps("psCb", [128, 512])
            P.op('dve', lambda e: e.memset(CARRY[:], 0.0), writes=['CARRY'])
            for tt in range(NT):
                P.op('dve', lambda e: e.tensor_tensor(out=MSUM[:], in0=MK[:, tt, 0, :], in1=MK[:, tt, 1, :], op=ALU.add), reads=['MK'], writes=['MSUM'])
                P.op('pe', lambda e: e.matmul(psK[:, 0:64], lhsT=TRI, rhs=MSUM[:], start=True, stop=False), reads=['CONST', 'MSUM'], writes=['psK'])
                P.op('pe', lambda e: e.matmul(psK[:, 0:64], lhsT=ONES[0:1, :], rhs=CARRY[0:1, :], start=False, stop=True), reads=['CONST', 'CARRY'], writes=['psK'])
                for k in range(2):
                    P.op('dve', lambda e: e.tensor_tensor(out=T64r[:], in0=MK[:, tt, k, :], in1=psK[:, 0:64], op=ALU.mult), reads=['MK', 'psK'], writes=['T64'])
                    P.op('dve', lambda e: e.reduce_sum(out=RSEL[:, tt, k:k + 1], in_=T64r[:], axis=AX), reads=['T64'], writes=['RSEL'])
                P.op('pe', lambda e: e.matmul(psC[0:1, 0:64], lhsT=ONES[:, 0:1], rhs=MSUM[:], start=True, stop=True), reads=['CONST', 'MSUM'], writes=['psC'])
                P.op('dve', lambda e: e.tensor_tensor(out=CARRY[0:1, :], in0=CARRY[0:1, :], in1=psC[0:1, 0:64], op=ALU.add), reads=['CARRY', 'psC'], writes=['CARRY'])
    if BACK:
        with P.scope():
            CI = P.sb("CI", [1, 64], I32); PAD = P.sb("PAD", [1, 64], F32)
            PE_ = [P.sb(f"PE{i}", [1, 64], F32) for i in range(2)]
            PST = P.sb("PST", [1, 64], F32); PSB = P.sb("PSB", [128, 64], F32); T64 = P.sb("T64b", [128, 64], F32)
            PSTK = P.sb("PSTK", [128, 1], F32); DF = P.sb("DF", [128, NT, 2], F32)
            PEC = P.sb("PEC", [64, 1], F32); BG = P.sb("BG", [64, NB], F32); CM = P.sb("CM", [64, NB], F32)
            BEF = P.sb("BEF", [128, NB], F32); IOP = P.sb("IOP", [128, 1], F32); ONE1 = P.sb("ONE1", [1, 1], F32)
            WQ = P.sb("WQ", [128, NB], F32)
            FILL = P.sb("FILL", [128, NB * 16], I32); TOKR = P.sb("TOKR", [128, NT, 16], I32)
            psd = P.ps("psd", [128, 512])
            P.dma('sp', BG[:], bgrid[:, :], writes=['BG']); P.dma('sp', IOP[:], iotap[:, :], writes=['IOP'])
            P.dma('sp', TOKR[:], tokrow[:, :, :], writes=['TOKR'])
            P.op('dve', lambda e: e.memset(ONE1[:], 1.0), writes=['ONE1'])
            P.op('pool', lambda e: e.memset(FILL[:], T), writes=['FILL'])
            P.dma('sp', SLOTTOK.rearrange("(p b) c -> p (b c)", p=128), FILL[:], reads=['FILL'], writes=['SLd'])
            P.op('dve', lambda e: e.tensor_scalar(out=CI[:], in0=CARRY[:], scalar1=127.0, scalar2=None, op0=ALU.add), reads=['CARRY'], writes=['CI'])
            P.op('dve', lambda e: e.tensor_scalar(out=CI[:], in0=CI[:], scalar1=7, scalar2=None, op0=ALU.arith_shift_right), reads=['CI'], writes=['CI'])
            P.op('dve', lambda e: e.tensor_scalar(out=CI[:], in0=CI[:], scalar1=7, scalar2=None, op0=ALU.logical_shift_left), reads=['CI'], writes=['CI'])
            P.op('dve', lambda e: e.tensor_copy(out=PAD[:], in_=CI[:]), reads=['CI'], writes=['PAD'])
            P.op('dve', lambda e: e.tensor_copy(out=PE_[0][:], in_=PAD[:]), reads=['PAD'], writes=['PE0'])
            cur = 0
            for st in range(6):
                sh = 1 << st
                nxt = 1 - cur
                P.op('dve', lambda e: e.tensor_copy(out=PE_[nxt][:, 0:sh], in_=PE_[cur][:, 0:sh]), reads=[f"PE{cur}"], writes=[f"PE{nxt}"])
                P.op('dve', lambda e: e.tensor_tensor(out=PE_[nxt][:, sh:64], in0=PE_[cur][:, sh:64], in1=PE_[cur][:, 0:64 - sh], op=ALU.add),
                     reads=[f"PE{cur}"], writes=[f"PE{nxt}"])
                cur = nxt
            PEND = PE_[cur]; pk = f"PE{cur}"
            P.op('dve', lambda e: e.tensor_tensor(out=PST[:], in0=PEND[:], in1=PAD[:], op=ALU.subtract), reads=[pk, 'PAD'], writes=['PST'])
            P.op('pe', lambda e: e.matmul(psd[:, 0:64], lhsT=ONES[0:1, :], rhs=PST[0:1, :], start=True, stop=True), reads=['CONST', 'PST'], writes=['psd'])
            P.op('act', lambda e: e.activation(out=PSB[:], in_=psd[:, 0:64], func=AF.Copy), reads=['psd'], writes=['PSB'])
            for tt in range(NT):
                for k in range(2):
                    P.op('dve', lambda e: e.tensor_tensor(out=T64[:], in0=MK[:, tt, k, :], in1=PSB[:], op=ALU.mult), reads=['MK', 'PSB'], writes=['T64b'])
                    P.op('dve', lambda e: e.reduce_sum(out=PSTK[:], in_=T64[:], axis=AX), reads=['T64b'], writes=['PSTK'])
                    P.op('dve', lambda e: e.tensor_tensor(out=DF[:, tt, k:k + 1], in0=PSTK[:], in1=RSEL[:, tt, k:k + 1], op=ALU.add), reads=['PSTK', 'RSEL'], writes=['DF'])
            P.op('dve', lambda e: e.tensor_copy(out=DESTI[:], in_=DF[:]), reads=['DF'], writes=['DESTI'])
            P.op('pe', lambda e: e.matmul(psd[0:64, 64:65], lhsT=PEND[0:1, :], rhs=ONE1[0:1, 0:1], start=True, stop=True), reads=[pk, 'ONE1', 'PSB'], writes=['psd'])
            P.op('act', lambda e: e.activation(out=PEC[:], in_=psd[0:64, 64:65], func=AF.Copy), reads=['psd'], writes=['PEC'])
            P.op('dve', lambda e: e.tensor_scalar(out=CM[:], in0=BG[:], scalar1=PEC[:, 0:1], scalar2=None, op0=ALU.is_ge), reads=['BG', 'PEC'], writes=['CM'])
            P.op('pe', lambda e: e.matmul(psd[:, 128:128 + NB], lhsT=ONES[0:64, :], rhs=CM[:], start=True, stop=True), reads=['CONST', 'CM', 'PEC'], writes=['psd'])
            P.op('dve', lambda e: e.tensor_scalar(out=BEF[:], in0=psd[:, 128:128 + NB], scalar1=63.0, scalar2=128.0, op0=ALU.min, op1=ALU.mult), reads=['psd'], writes=['BEF'])
            P.op('dve', lambda e: e.tensor_scalar(out=BEF[:], in0=BEF[:], scalar1=IOP[:, 0:1], scalar2=None, op0=ALU.add), reads=['BEF', 'IOP'], writes=['BEF'])
            for kq in range(4):
                P.op('dve', lambda e: e.tensor_scalar(out=WQ[:], in0=BEF[:], scalar1=4.0, scalar2=float(kq), op0=ALU.mult, op1=ALU.add), reads=['BEF'], writes=['WQ'])
                P.op('dve', lambda e: e.tensor_copy(out=WINI[:, :, kq], in_=WQ[:]), reads=['WQ'], writes=['WINI'])
            for h in range(2):
                P.op('dve', lambda e: e.tensor_scalar(out=WQ[:], in0=BEF[:], scalar1=2.0, scalar2=float(h), op0=ALU.mult, op1=ALU.add), reads=['BEF'], writes=['WQ'])
                P.op('dve', lambda e: e.tensor_copy(out=WOUTI[:, :, h], in_=WQ[:]), reads=['WQ'], writes=['WOUTI'])
            for tt in range(NT):
                for k in range(2):
                    P.idma(SLOTTOK[:, :], TOKR[:, tt, :], out_idx=DESTI[:, tt, k:k + 1], reads=['TOKR', 'DESTI', 'SLd'], writes=[f"SLs{tt}_{k}"])
        MKs.close()
        STB = P.sb("STB", [128, NB, 16], I32)
        with P.scope():
            XB = P.sb("XB", [128, D], F32); XBT = P.sb("XBT", [128, KC, 128], BF16)
            WW = [P.sb(f"WW{i}", [128, 8192], F32) for i in range(2)]
            WB = [P.sb(f"WBe{i}", [128, 8192], BF16) for i in range(3)]
            CENG = ['act', 'dve', 'act', 'dve', 'act', 'dve']
            SG = P.sb("SG", [128, 512], F32); AV = P.sb("AV", [128, 512], F32); AT = P.sb("AT", [128, 4, 128], BF16)
            YB = P.sb("YB", [128, D], BF16)
            psT = [P.ps(f"psT{i}", [128, 512]) for i in range(2)]
            psH = [P.ps(f"psH{i}", [128, 512]) for i in range(2)]
            psO = [P.ps(f"psO{i}", [128, 512]) for i in range(2)]
            P.dma('sp', STB[:], SLOTTOK.rearrange("(b p) c -> p b c", p=128), reads=['SLd'] + [f"SLs{t_}_{k_}" for t_ in range(NT) for k_ in range(2)], writes=['STB'])
            wn = 0

            def emit_G(b):
                P.idma(XB[:], HN[:, :], in_idx=STB[:, b, 0:1], reads=['STB', 'HNd'], writes=['XB'])

            def emit_T(b):
                for k4 in range(8):
                    pt = psT[k4 % 2]
                    for q in range(4):
                        kc = k4 * 4 + q
                        P.op('pe', lambda e: e.transpose(out=pt[:, q * 128:(q + 1) * 128], in_=XB[:, kc * 128:(kc + 1) * 128], identity=IDENT),
                             reads=['XB', 'CONST'], writes=[f"psT{k4 % 2}"])
                    dstv = XBT[:, k4 * 4:(k4 + 1) * 4, :].rearrange("p a t -> p (a t)")
                    if k4 % 2 == 0:
                        P.op('act', lambda e: e.activation(out=dstv, in_=pt[:, :], func=AF.Copy), reads=[f"psT{k4 % 2}"], writes=['XBT'])
                    else:
                        P.op('dve', lambda e: e.tensor_copy(out=dstv, in_=pt[:, :]), reads=[f"psT{k4 % 2}"], writes=['XBT'])

            emit_G(0)
            emit_T(0)
            for b in range(NB):
                if b + 1 < NB:
                    emit_G(b + 1)
                for kq in range(4):
                    eng = CENG[wn % 6]; wi = wn % 2; bi = wn % 3; wn += 1
                    P.idma(WW[wi][:], ewin[:, :], in_idx=WINI[:, b, kq:kq + 1], reads=['WINI'], writes=[f"WW{wi}"])
                    if eng == 'act':
                        P.op('act', lambda e: e.activation(out=WB[bi][:], in_=WW[wi][:], func=AF.Copy), reads=[f"WW{wi}"], writes=[f"WBe{bi}"])
                    else:
                        P.op(eng, lambda e: e.tensor_copy(out=WB[bi][:], in_=WW[wi][:]), reads=[f"WW{wi}"], writes=[f"WBe{bi}"])
                    for kcl in range(8):
                        kc = kq * 8 + kcl
                        for hf in range(2):
                            P.op('pe', lambda e: e.matmul(psH[hf][:, :], lhsT=XBT[:, kc, :], rhs=WB[bi][:, kcl * 1024 + hf * 512:kcl * 1024 + (hf + 1) * 512],
                                                          start=(kc == 0), stop=(kc == KC - 1)),
                                 reads=['XBT', f"WBe{bi}"], writes=[f"psH{hf}"])
                P.op('act', lambda e: e.activation(out=SG[:], in_=psH[0][:, :], func=AF.Silu), reads=["psH0"], writes=['SG'])
                P.op('dve', lambda e: e.tensor_tensor(out=AV[:], in0=SG[:], in1=psH[1][:, :], op=ALU.mult), reads=['SG', "psH1"], writes=['AV'])
                if b + 1 < NB:
                    emit_T(b + 1)
                for q in range(4):
                    P.op('pe', lambda e: e.transpose(out=psT[0][:, q * 128:(q + 1) * 128], in_=AV[:, q * 128:(q + 1) * 128], identity=IDENT),
                         reads=['AV', 'CONST'], writes=["psT0"])
                P.op('act', lambda e: e.activation(out=AT[:].rearrange("p a t -> p (a t)"), in_=psT[0][:, :], func=AF.Copy), reads=["psT0"], writes=['AT'])
                for h in range(2):
                    eng = CENG[wn % 6]; wi = wn % 2; bi = wn % 3; wn += 1
                    P.idma(WW[wi][:], ewout[:, :], in_idx=WOUTI[:, b, h:h + 1], reads=['WOUTI'], writes=[f"WW{wi}"])
                    if eng == 'act':
                        P.op('act', lambda e: e.activation(out=WB[bi][:], in_=WW[wi][:], func=AF.Copy), reads=[f"WW{wi}"], writes=[f"WBe{bi}"])
                    else:
                        P.op(eng, lambda e: e.tensor_copy(out=WB[bi][:], in_=WW[wi][:]), reads=[f"WW{wi}"], writes=[f"WBe{bi}"])
                    for nb_ in range(4):
                        po = (h * 4 + nb_) % 2
                        for kq in range(4):
                            P.op('pe', lambda e: e.matmul(psO[po][:, :], lhsT=AT[:, kq, :], rhs=WB[bi][:, kq * 2048 + nb_ * 512:kq * 2048 + (nb_ + 1) * 512],
                                                          start=(kq == 0), stop=(kq == 3)),
                                 reads=['AT', f"WBe{bi}"], writes=[f"psO{po}"])
                        dsl = slice(h * 2048 + nb_ * 512, h * 2048 + (nb_ + 1) * 512)
                        if nb_ % 2 == 0:
                            P.op('act', lambda e: e.activation(out=YB[:, dsl], in_=psO[po][:, :], func=AF.Copy), reads=[f"psO{po}"], writes=['YB'])
                        else:
                            P.op('dve', lambda e: e.tensor_copy(out=YB[:, dsl], in_=psO[po][:, :]), reads=[f"psO{po}"], writes=['YB'])
                P.dma('sp', YS[b * 128:(b + 1) * 128, :], YB[:], reads=['YB'], writes=['YSd'])
        with P.scope():
            XR = [P.sb(f"XR{i}", [128, D], F32) for i in range(2)]
            Y0 = [P.sb(f"Y0{i}", [128, D], BF16) for i in range(2)]
            Y1 = [P.sb(f"Y1{i}", [128, D], BF16) for i in range(2)]
            JUNK = P.sb("JUNK", [128, D], BF16)
            SS = P.sb("SS", [128, 1], F32); RS = P.sb("RS", [128, 1], F32); EPSC = P.sb("EPSC", [128, 1], F32)
            ROWT = P.sb("ROWT", [1, D], F32)
            psl = P.ps("psl", [128, 512])
            P.op('dve', lambda e: e.memset(EPSC[:], EPS), writes=['EPSC'])
            if final:
                bcast_row(GBC, 'GBC', grows[1:2, :], D, psl, ROWT)
            for tt in range(NT):
                i = tt % 2
                tsl = slice(tt * 128, (tt + 1) * 128)
                P.dma('sp', XR[i][:], X1[tsl, :], writes=[f"XR{i}"])
                P.idma(Y0[i][:], YS[:, :], in_idx=DESTI[:, tt, 0:1], reads=['DESTI'], writes=[f"Y0{i}"])
                P.idma(Y1[i][:], YS[:, :], in_idx=DESTI[:, tt, 1:2], reads=['DESTI'], writes=[f"Y1{i}"])
                P.op('dve', lambda e: e.scalar_tensor_tensor(out=XR[i][:], in0=Y0[i][:], scalar=GATES[:, tt, 0:1], in1=XR[i][:], op0=ALU.mult, op1=ALU.add),
                     reads=[f"Y0{i}", 'GATES', f"XR{i}"], writes=[f"XR{i}"])
                P.op('dve', lambda e: e.scalar_tensor_tensor(out=XR[i][:], in0=Y1[i][:], scalar=GATES[:, tt, 1:2], in1=XR[i][:], op0=ALU.mult, op1=ALU.add),
                     reads=[f"Y1{i}", 'GATES', f"XR{i}"], writes=[f"XR{i}"])
                if final:
                    P.op('act', lambda e: e.activation(out=JUNK[:], in_=XR[i][:], func=AF.Square, accum_out=SS[:, 0:1]), reads=[f"XR{i}"], writes=['JUNK', 'SS'])
                    P.op('act', lambda e: e.activation(out=RS[:], in_=SS[:], func=AF.Ln, bias=EPSC[:, 0:1], scale=1.0 / D), reads=['SS', 'EPSC'], writes=['RS'])
                    P.op('act', lambda e: e.activation(out=RS[:], in_=RS[:], func=AF.Exp, scale=-0.5), reads=['RS'], writes=['RS'])
                    P.op('dve', lambda e: e.scalar_tensor_tensor(out=XR[i][:], in0=XR[i][:], scalar=RS[:, 0:1], in1=GBC[:], op0=ALU.mult, op1=ALU.mult),
                         reads=[f"XR{i}", 'RS', 'GBC'], writes=[f"XR{i}"])
                P.dma('act', out[tsl, :], XR[i][:], reads=[f"XR{i}"], is_output=True)

    if not BACK:
        P.barrier()
        MKs.close()
    P.finish('sp')
    print("L3 ninstr", P.ninstr, "nsem", P.nsem)
    es.close()
    return nc

def l3_inputs(L, xT, mixT, w, T):
    f32 = np.float32
    NB = n_moe_blocks(T); NT = T // 128
    cat = np.concatenate([w['router_group_w'][L], w['router_expert_w'][L]], 1)
    m = {"xT": xT, "mixT": mixT,
         "cvcols": np.stack([w['conv_ln_g'][L].reshape(8, 128).T, w['conv_ln_b'][L].reshape(8, 128).T], 1),
         "pwt": w['conv_pw'][L].reshape(8, 128, 8, 128).transpose(2, 1, 0, 3),
         "wot": w['w_out'][L].reshape(32, 128, 32, 128).transpose(2, 1, 0, 3),
         "fgain": w['ffn_norm'][L].reshape(32, 128).T,
         "grows": np.stack([w['ffn_norm'][L], w['final_norm']], 0),
         "wr": cat.reshape(32, 128, 72).transpose(1, 0, 2),
         "rbias": np.concatenate([w['router_group_b'][L], w['router_expert_b'][L]])[None, :],
         "ewin": w['expert_w_in'][L].reshape(64, 4, 8, 128, 1024).transpose(0, 3, 1, 2, 4).reshape(64 * 128 * 4, 8192),
         "ewout": w['expert_w_out'][L].reshape(64, 4, 128, 2, 2048).transpose(0, 2, 3, 1, 4).reshape(64 * 128 * 2, 8192),
         "consts": np.stack([np.ones((128, 128), f32), np.eye(128, dtype=f32), np.triu(np.ones((128, 128), f32), 1)], 0),
         "bgrid": np.broadcast_to(128.0 * np.arange(NB, dtype=f32)[None, :], (64, NB)),
         "iotap": np.arange(128, dtype=f32)[:, None]}
    m = {k: np.ascontiguousarray(v, dtype=f32) for k, v in m.items()}
    m["tokrow"] = np.ascontiguousarray(np.broadcast_to((np.arange(NT, dtype=np.int32)[None, :, None] * 128 + np.arange(128, dtype=np.int32)[:, None, None]), (128, NT, 16)))
    return m


def kernel(**inputs):
    w = {k: np.asarray(v) for k, v in inputs.items()}
    x = np.asarray(w['x'][0], dtype=np.float32)
    T = x.shape[0]
    TL = T // 8
    xT = np.ascontiguousarray(x.T)
    vT = None
    x2 = None
    FK = ["consts", "grows", "cvcols", "pwt", "wot", "fgain", "wr", "rbias"]
    BK = ["consts", "grows", "ewin", "ewout", "bgrid", "iotap", "tokrow"]
    for L in range(2):
        nc1 = build_l1(T, L > 0)
        maps = []
        for c in range(8):
            m = l1_core_inputs(c, xT, L, w)
            if L > 0:
                m["vfT"] = vT[c]
            maps.append(m)
        r = run_bass_kernel_spmd(nc1, maps, core_ids=list(range(8))).results
        del maps
        mixT = np.empty((4096, T), np.float32)
        for c in range(8):
            g, half = c // 2, c % 2
            mixT[g * 256 + half * 128:g * 256 + (half + 1) * 128] = r[c]["ypoolT"]
            mixT[1024 + c * 128:1024 + (c + 1) * 128] = r[c]["hcT"]
            mixT[2048 + c * 256:2048 + (c + 1) * 256] = r[c]["yrwT"]
        if L == 0:
            vT = [np.ascontiguousarray(r[c]["vT"], dtype=np.float32) for c in range(8)]
        del r
        m3 = l3_inputs(L, xT, mixT, w, T)
        nc2 = build_l3(TL, False, 'front')
        maps = []
        for c in range(8):
            mm = {kk: m3[kk] for kk in FK}
            mm["xT"] = np.ascontiguousarray(xT[:, c * TL:(c + 1) * TL])
            mm["mixT"] = np.ascontiguousarray(mixT[:, c * TL:(c + 1) * TL])
            maps.append(mm)
        r = run_bass_kernel_spmd(nc2, maps, core_ids=list(range(8))).results
        del maps
        mb = {kk: m3[kk] for kk in BK}
        del m3
        mb["x1_in"] = np.ascontiguousarray(np.concatenate([r[c]["x1_out"] for c in range(8)], 0), dtype=np.float32)
        mb["hn_in"] = np.ascontiguousarray(np.concatenate([r[c]["hn_out"] for c in range(8)] + [np.zeros((128, 4096), np.float32)], 0), dtype=np.float32)
        mb["mk_in"] = np.ascontiguousarray(np.concatenate([r[c]["mk_out"] for c in range(8)], 1), dtype=np.float32)
        mb["gates_in"] = np.ascontiguousarray(np.concatenate([r[c]["gates_out"] for c in range(8)], 1), dtype=np.float32)
        del r
        nc3 = build_l3(T, L == 1, 'back')
        x2 = np.asarray(run_bass_kernel_spmd(nc3, [mb], core_ids=[0]).results[0]["out"], dtype=np.float32)
        del mb
        xT = np.ascontiguousarray(x2.T)
    return x2[None]
```
